# Optimizing a Trainium2 kernel written in Bass

```python
import math
import jax, jax.numpy as jnp
from jax import lax
import numpy as np

D_MODEL = 2048
BATCH = 4
SEQ = 2048
DEPTH = 1

ATTN_HEADS = 8
HEAD_DIM = 128
ATTN_WIDTH = ATTN_HEADS * HEAD_DIM
MOBA_BLOCK = 256
MOBA_TOPK = 3
Q_CHUNK = 32
LRU_WIDTH = 1024
LRU_BLOCKS = 8
LRU_BLOCK_DIM = LRU_WIDTH // LRU_BLOCKS
CONV_WIDTH = 4
LRU_C = 8.0
N_BRANCHES = 2
IN_WIDTH = 3 * ATTN_WIDTH + 2 * LRU_WIDTH + N_BRANCHES * D_MODEL
REL_BUCKETS = 32
REL_MAX_DIST = 128
PEER_HEADS = 8
PEER_NKEYS = 128
PEER_EXPERTS = PEER_NKEYS * PEER_NKEYS
PEER_DKEY = 256
PEER_TOPK = 16
PEER_CHUNK = 128
EPS = 1e-6
NEG = -1e30

kernel_name = 'hybrid_moba_rglru_peer'


def rmsnorm(x, g):
    xf = x.astype(jnp.float32)
    y = xf * lax.rsqrt(jnp.mean(xf * xf, axis=-1, keepdims=True) + EPS)
    return (y * g.astype(jnp.float32)).astype(x.dtype)


def rel_bucket(dist):
    n = jnp.maximum(dist, 0)
    max_exact = REL_BUCKETS // 2
    nf = jnp.maximum(n, 1).astype(jnp.float32)
    large = max_exact + (jnp.log(nf / max_exact) / math.log(REL_MAX_DIST / max_exact)
                         * (REL_BUCKETS - max_exact)).astype(jnp.int32)
    large = jnp.minimum(large, REL_BUCKETS - 1)
    return jnp.where(n < max_exact, n, large)


def moba_attention(q, k, v, rel_bias):
    B, H, S, hd = q.shape
    nb = -(-S // MOBA_BLOCK)
    s_pad = nb * MOBA_BLOCK
    topk = min(MOBA_TOPK, nb)
    pad = ((0, 0), (0, 0), (0, s_pad - S), (0, 0))
    k_pad = jnp.pad(k, pad)
    v_pad = jnp.pad(v, pad)
    k_blocks = k_pad.reshape(B, H, nb, MOBA_BLOCK, hd)
    v_blocks = v_pad.reshape(B, H, nb, MOBA_BLOCK, hd)
    k_mean = jnp.mean(k_blocks.astype(jnp.float32), axis=3)
    bias_h = rel_bias.T.astype(jnp.float32)
    scale = HEAD_DIM ** -0.5
    b_idx = jnp.arange(B)[:, None, None, None]
    h_idx = jnp.arange(H)[None, :, None, None]
    h_idx5 = jnp.arange(H)[None, :, None, None, None]
    blk_ar = jnp.arange(nb)
    slot_ar = jnp.arange(topk)
    in_blk = jnp.arange(MOBA_BLOCK)

    def chunk(start):
        qs = lax.dynamic_slice_in_dim(q, start, Q_CHUNK, axis=2).astype(jnp.float32)
        q_pos = start + jnp.arange(Q_CHUNK)
        own = start // MOBA_BLOCK
        gate = jnp.einsum('bhqd,bhnd->bhqn', qs, k_mean)
        gate = jnp.where(blk_ar < own, gate, NEG)
        _, sel = lax.top_k(gate, topk)
        slot_ok = slot_ar < own
        k_sel = k_blocks[b_idx, h_idx, sel].astype(jnp.float32)
        v_sel = v_blocks[b_idx, h_idx, sel].astype(jnp.float32)
        k_pos_sel = sel[..., None] * MOBA_BLOCK + in_blk
        dist_sel = q_pos[:, None, None] - k_pos_sel
        bias_sel = bias_h[h_idx5, rel_bucket(dist_sel)]
        logit_sel = jnp.einsum('bhqd,bhqjkd->bhqjk', qs, k_sel) * scale + bias_sel
        logit_sel = jnp.where(slot_ok[:, None], logit_sel, NEG)
        k_own = lax.dynamic_slice_in_dim(k_pad, own * MOBA_BLOCK, MOBA_BLOCK, axis=2).astype(jnp.float32)
        v_own = lax.dynamic_slice_in_dim(v_pad, own * MOBA_BLOCK, MOBA_BLOCK, axis=2).astype(jnp.float32)
        dist_own = q_pos[:, None] - (own * MOBA_BLOCK + in_blk)[None, :]
        bias_own = bias_h[:, rel_bucket(dist_own)]
        logit_own = jnp.einsum('bhqd,bhkd->bhqk', qs, k_own) * scale + bias_own
        logit_own = jnp.where(dist_own >= 0, logit_own, NEG)
        logits = jnp.concatenate(
            [logit_sel.reshape(B, H, Q_CHUNK, topk * MOBA_BLOCK), logit_own], axis=-1)
        p = jax.nn.softmax(logits, axis=-1)
        p_sel = p[..., :topk * MOBA_BLOCK].reshape(B, H, Q_CHUNK, topk, MOBA_BLOCK)
        p_own = p[..., topk * MOBA_BLOCK:]
        out = (jnp.einsum('bhqjk,bhqjkd->bhqd', p_sel, v_sel)
               + jnp.einsum('bhqk,bhkd->bhqd', p_own, v_own))
        return out.astype(q.dtype)

    starts = jnp.arange(S // Q_CHUNK, dtype=jnp.int32) * Q_CHUNK
    out = lax.map(chunk, starts)
    return out.transpose(1, 0, 3, 2, 4).reshape(B, S, H * hd)


def causal_conv(x, w, b):
    C = x.shape[-1]
    y = lax.conv_general_dilated(
        x, w[:, None, :].astype(x.dtype), window_strides=(1,),
        padding=[(CONV_WIDTH - 1, 0)], dimension_numbers=('NWC', 'WIO', 'NWC'),
        feature_group_count=C)
    return y + b.astype(x.dtype)


def rg_lru(x, w_a, b_a, w_x, b_x, lam):
    B, S, C = x.shape
    xf = x.astype(jnp.float32)
    xb = xf.reshape(B, S, LRU_BLOCKS, LRU_BLOCK_DIM)
    r = jax.nn.sigmoid(jnp.einsum('bsgi,gij->bsgj', xb, w_a.astype(jnp.float32)).reshape(B, S, C)
                       + b_a.astype(jnp.float32))
    i = jax.nn.sigmoid(jnp.einsum('bsgi,gij->bsgj', xb, w_x.astype(jnp.float32)).reshape(B, S, C)
                       + b_x.astype(jnp.float32))
    log_a = -LRU_C * r * jax.nn.softplus(-lam.astype(jnp.float32))
    a = jnp.exp(log_a)
    u = jnp.sqrt(-jnp.expm1(2.0 * log_a)) * (i * xf)

    def combine(left, right):
        a1, b1 = left
        a2, b2 = right
        return a1 * a2, a2 * b1 + b2

    _, h = lax.associative_scan(combine, (a, u), axis=1)
    return h.astype(x.dtype)


def peer(x, w_q, sub_keys, u_tab, v_tab):
    B, S, D = x.shape
    T = B * S
    K = PEER_TOPK
    xt = x.reshape(T, D)
    q = (xt @ w_q).astype(jnp.float32).reshape(T, PEER_HEADS, 2, PEER_DKEY // 2)
    s = jnp.einsum('thcd,hcnd->thcn', q, sub_keys.astype(jnp.float32))
    sv, si = lax.top_k(s, K)
    cand = sv[:, :, 0, :, None] + sv[:, :, 1, None, :]
    cv, ci = lax.top_k(cand.reshape(T, PEER_HEADS, K * K), K)
    i1 = jnp.take_along_axis(si[:, :, 0], ci // K, axis=-1)
    i2 = jnp.take_along_axis(si[:, :, 1], ci % K, axis=-1)
    idx = (i1 * PEER_NKEYS + i2).reshape(T, PEER_HEADS * K)
    g = jax.nn.softmax(cv, axis=-1).reshape(T, PEER_HEADS * K)
    n_c = T // PEER_CHUNK

    def chunk(args):
        xc, ic, gc = args
        u = u_tab[ic]
        act = jax.nn.gelu(jnp.einsum('td,ted->te', xc, u).astype(jnp.float32)) * gc
        v = v_tab[ic]
        return jnp.einsum('te,ted->td', act.astype(v.dtype), v)

    out = lax.map(chunk, (xt.reshape(n_c, PEER_CHUNK, D),
                          idx.reshape(n_c, PEER_CHUNK, PEER_HEADS * K),
                          g.reshape(n_c, PEER_CHUNK, PEER_HEADS * K)))
    return out.reshape(B, S, D).astype(x.dtype)


def setup_inputs(seed: int = 0) -> dict:
    key = jax.random.key(seed)
    ks = jax.random.split(key, 20)

    def nrm(k, shape, scale):
        return jax.random.normal(k, shape, jnp.float32) * scale

    x = nrm(ks[0], (BATCH, SEQ, D_MODEL), 1.0)
    norm_mix_g = 1.0 + nrm(ks[1], (DEPTH, D_MODEL), 0.02)
    w_in = nrm(ks[2], (DEPTH, D_MODEL, IN_WIDTH), D_MODEL ** -0.5)
    conv_w = nrm(ks[3], (DEPTH, CONV_WIDTH, LRU_WIDTH), CONV_WIDTH ** -0.5)
    conv_b = nrm(ks[4], (DEPTH, LRU_WIDTH), 0.01)
    lru_wa = nrm(ks[5], (DEPTH, LRU_BLOCKS, LRU_BLOCK_DIM, LRU_BLOCK_DIM), LRU_BLOCK_DIM ** -0.5)
    lru_ba = nrm(ks[6], (DEPTH, LRU_WIDTH), 0.01)
    lru_wx = nrm(ks[7], (DEPTH, LRU_BLOCKS, LRU_BLOCK_DIM, LRU_BLOCK_DIM), LRU_BLOCK_DIM ** -0.5)
    lru_bx = nrm(ks[8], (DEPTH, LRU_WIDTH), 0.01)
    a_c = jax.random.uniform(ks[9], (DEPTH, LRU_WIDTH), jnp.float32, 0.9, 0.999)
    p = a_c ** (1.0 / LRU_C)
    lru_lambda = jnp.log(p) - jnp.log1p(-p)
    w_branch = nrm(ks[10], (DEPTH, N_BRANCHES, ATTN_WIDTH, D_MODEL), ATTN_WIDTH ** -0.5)
    w_out = nrm(ks[11], (DEPTH, D_MODEL, D_MODEL), D_MODEL ** -0.5)
    rel_bias = nrm(ks[12], (REL_BUCKETS, ATTN_HEADS), 0.1)
    norm_ffn_g = 1.0 + nrm(ks[13], (DEPTH, D_MODEL), 0.02)
    peer_wq = nrm(ks[14], (DEPTH, D_MODEL, PEER_HEADS * PEER_DKEY), D_MODEL ** -0.5)
    peer_keys = nrm(ks[15], (DEPTH, PEER_HEADS, 2, PEER_NKEYS, PEER_DKEY // 2), (PEER_DKEY // 2) ** -0.5)
    peer_u = nrm(ks[16], (DEPTH, PEER_EXPERTS, D_MODEL), D_MODEL ** -0.5)
    peer_v = nrm(ks[17], (DEPTH, PEER_EXPERTS, D_MODEL), PEER_HEADS ** -0.5)
    norm_final_g = 1.0 + nrm(ks[18], (D_MODEL,), 0.02)
    return {'x': x, 'norm_mix_g': norm_mix_g, 'w_in': w_in, 'conv_w': conv_w, 'conv_b': conv_b,
            'lru_wa': lru_wa, 'lru_ba': lru_ba, 'lru_wx': lru_wx, 'lru_bx': lru_bx,
            'lru_lambda': lru_lambda, 'w_branch': w_branch, 'w_out': w_out, 'rel_bias': rel_bias,
            'norm_ffn_g': norm_ffn_g, 'peer_wq': peer_wq, 'peer_keys': peer_keys,
            'peer_u': peer_u, 'peer_v': peer_v, 'norm_final_g': norm_final_g}


def reference(x, norm_mix_g, w_in, conv_w, conv_b, lru_wa, lru_ba, lru_wx, lru_bx, lru_lambda,
              w_branch, w_out, rel_bias, norm_ffn_g, peer_wq, peer_keys, peer_u, peer_v, norm_final_g):
    B, S, _ = x.shape
    splits = [ATTN_WIDTH, 2 * ATTN_WIDTH, 3 * ATTN_WIDTH,
              3 * ATTN_WIDTH + LRU_WIDTH, 3 * ATTN_WIDTH + 2 * LRU_WIDTH]
    for l in range(DEPTH):
        h = rmsnorm(x, norm_mix_g[l])
        proj = h @ w_in[l]
        q, k, v, xr, yr, gl = jnp.split(proj, splits, axis=-1)
        q = q.reshape(B, S, ATTN_HEADS, HEAD_DIM).transpose(0, 2, 1, 3)
        k = k.reshape(B, S, ATTN_HEADS, HEAD_DIM).transpose(0, 2, 1, 3)
        v = v.reshape(B, S, ATTN_HEADS, HEAD_DIM).transpose(0, 2, 1, 3)
        o_att = moba_attention(q, k, v, rel_bias)
        xr = causal_conv(xr, conv_w[l], conv_b[l])
        o_rec = rg_lru(xr, lru_wa[l], lru_ba[l], lru_wx[l], lru_bx[l], lru_lambda[l]) * jax.nn.gelu(yr)
        branches = jnp.stack([o_att, o_rec], axis=2)
        pb = jnp.einsum('bsnc,ncd->bsnd', branches, w_branch[l])
        gates = jax.nn.sigmoid(gl.astype(jnp.float32)).reshape(B, S, N_BRANCHES, D_MODEL)
        merged = jnp.sum(gates.astype(pb.dtype) * pb, axis=2)
        x = x + merged @ w_out[l]
        x = x + peer(rmsnorm(x, norm_ffn_g[l]), peer_wq[l], peer_keys[l], peer_u[l], peer_v[l])
    return rmsnorm(x, norm_final_g)
```

```python
import math
from contextlib import ExitStack
import numpy as np
import concourse.bass as bass
import concourse.mybir as mybir
from concourse.bass_utils import run_bass_kernel_spmd

F32 = mybir.dt.float32
BF16 = mybir.dt.bfloat16
U32 = mybir.dt.uint32
AF = mybir.ActivationFunctionType
ALU = mybir.AluOpType
AX = mybir.AxisListType

NEG = -1e30
SCALE = 128 ** -0.5
DEBUG = {}


class Buf:
    __slots__ = ("w", "r", "name")

    def __init__(self, name=""):
        self.w = None
        self.r = {}
        self.name = name


class Eng:
    def __init__(self, name, eng, sem, sid, is_pe=False):
        self.name, self.eng, self.sem, self.sid = name, eng, sem, sid
        self.n = 0
        self.waited = {}
        self.is_pe = is_pe


class FW:
    def __init__(self, nc, es):
        self.nc = nc
        self.es = es
        self.sems = {}
        self.engs = {}
        sid = 0
        for name, eng, pe in (("pe", nc.tensor, True), ("act", nc.scalar, False),
                              ("dve", nc.vector, False), ("pool", nc.gpsimd, False)):
            sem = es.enter_context(nc.semaphore("s_" + name))
            self.sems[sid] = sem
            self.engs[name] = Eng(name, eng, sem, sid, pe)
            sid += 1
        self.queues = {}
        for qname, eng, npool in (("sp", nc.sync, 24), ("gq", nc.gpsimd, 8)):
            pool = []
            for i in range(npool):
                sem = es.enter_context(nc.semaphore(f"d_{qname}{i}"))
                self.sems[sid] = sem
                pool.append([sid, 0])
                sid += 1
            self.queues[qname] = dict(eng=eng, pool=pool, k=0, waited={}, name=qname)
        self.queues["gq"]["waited"] = self.engs["pool"].waited
        self.nsem = sid

    def _wait(self, eng_obj, waited, deps):
        for sid, val in deps.items():
            if waited.get(sid, 0) < val:
                eng_obj.wait_ge(self.sems[sid], val)
                waited[sid] = val

    def _deps(self, reads, writes, self_sid, is_pe):
        deps = {}

        def add(ev):
            sid, val = ev
            if deps.get(sid, 0) < val:
                deps[sid] = val
        for b in reads:
            if b.w is not None:
                if not (is_pe and b.w[0] == self_sid):
                    add(b.w)
        for b in writes:
            if b.w is not None and not (is_pe and b.w[0] == self_sid):
                add(b.w)
            for sid, val in b.r.items():
                if not (is_pe and sid == self_sid):
                    add((sid, val))
        return deps

    def _mark(self, ev, reads, writes):
        for b in reads:
            if b.r.get(ev[0], 0) < ev[1]:
                b.r[ev[0]] = ev[1]
        for b in writes:
            b.w = ev
            b.r = {}

    def op(self, ename, fn, reads=(), writes=(), signal=True):
        e = self.engs[ename]
        deps = self._deps(reads, writes, e.sid, e.is_pe)
        if deps.get(e.sid, 0) > e.n:
            if e.n > 0:
                deps[e.sid] = e.n
            else:
                del deps[e.sid]
        self._wait(e.eng, e.waited, deps)
        ins = fn()
        if signal:
            e.n += 1
            ins.then_inc(e.sem, 1)
            ev = (e.sid, e.n)
        else:
            ev = (e.sid, e.n + 1)
        self._mark(ev, reads, writes)
        return ins

    def dma(self, qname, out, in_, reads=(), writes=()):
        q = self.queues[qname]
        slot = q["pool"][q["k"] % len(q["pool"])]
        q["k"] += 1
        deps = self._deps(reads, writes, -1, False)
        if slot[1] > 0:
            deps[slot[0]] = max(deps.get(slot[0], 0), slot[1])
        self._wait(q["eng"], q["waited"], deps)
        slot[1] += 16
        q["eng"].dma_start(out=out, in_=in_).then_inc(self.sems[slot[0]], 16)
        ev = (slot[0], slot[1])
        self._mark(ev, reads, writes)
        return ev

    def barrier(self):
        tot = {}
        for e in self.engs.values():
            if e.n:
                tot[e.sid] = e.n
        for q in self.queues.values():
            for sid, val in q["pool"]:
                if val:
                    tot[sid] = val
        for e in self.engs.values():
            self._wait(e.eng, e.waited, {s: v for s, v in tot.items() if s != e.sid or not e.is_pe})
        q = self.queues["sp"]
        self._wait(q["eng"], q["waited"], tot)


def build_program(dbg=None):
    nc = bass.Bass("TRN2", target_bir_lowering=False)
    es = ExitStack()
    fw = FW(nc, es)
    op, dma = fw.op, fw.dma

    def din(name, shape, dt=F32):
        return nc.dram_tensor(name, list(shape), dt, kind="ExternalInput").ap()

    xloc = din("xloc", [2048, 2048])
    win_g = din("win_g", [72, 128, 2048])
    wbr_g = din("wbr_g", [32, 128, 1024])
    wout_g = din("wout_g", [16, 128, 2048])
    wq_g = din("wq_g", [16, 128, 2048])
    NUV = 128 if DEBUG.get("stage", 99) >= 5 else 1
    ut_g = din("ut_g", [NUV, 128, 2048])
    v_g = din("v_g", [NUV, 128, 2048])
    keysT = din("keysT", [128, 16, 128])
    cst = din("cst", [128, 512])
    lruw = din("lruw", [128, 2, 8, 128])
    ident_d = din("ident", [128, 128])
    ownbias_d = din("ownbias", [128, 8, 2, 256])
    prevbias_d = din("prevbias", [128, 8, 256])
    blkvalid_d = din("blkvalid", [128, 8, 8])
    gfin_d = din("gfin", [128, 2048])
    iota_d = din("iota", [128, 128])
    y = nc.dram_tensor("y", [1024, 2048], F32, kind="ExternalOutput").ap()
    gscr = nc.dram_tensor("gscr", [128, 128, 1024], BF16, kind="Internal").ap()
    x2s = nc.dram_tensor("x2s", [1024, 2048], F32, kind="Internal").ap()
    x2s_b = Buf()
    gscr_b = Buf()
    dbg_out = {}
    if dbg:
        for k, shp in dbg.items():
            dbg_out[k] = nc.dram_tensor("dbg_" + k, list(shp), F32, kind="ExternalOutput").ap()

    def sb(name, shape, dt=F32, stack=es):
        return stack.enter_context(nc.sbuf_tensor(name, list(shape), dt))

    banks = [es.enter_context(nc.psum_tensor(f"ps{i}", [128, 512], F32)) for i in range(8)]
    bank_bufs = [Buf(f"ps{i}") for i in range(8)]
    bank_k = [0]

    def next_bank():
        i = bank_k[0] % 8
        bank_k[0] += 1
        return banks[i], bank_bufs[i]

    cst_sb = sb("cst_sb", [128, 512]); cst_b = Buf()
    ident_f = sb("ident_f", [128, 128]); ident_b = Buf()
    ident_h = sb("ident_h", [128, 128], BF16)
    dma("sp", cst_sb[:], cst[:, :], writes=[cst_b])
    dma("sp", ident_f[:], ident_d[:, :], writes=[ident_b])
    op("dve", lambda: nc.vector.tensor_copy(out=ident_h[:], in_=ident_f[:]), reads=[ident_b], writes=[ident_b])
    C_GMIX, C_GFFN, C_CONVW, C_CONVB, C_BA, C_BX, C_LAM, C_FLAG, C_B31 = 0, 16, 32, 64, 72, 80, 88, 96, 100
    gmix = cst_sb[:, C_GMIX:C_GMIX + 16]
    gffn = cst_sb[:, C_GFFN:C_GFFN + 16]

    NST = 3
    NSF = 2
    wst = [sb(f"wst{i}", [128, 2048]) for i in range(NSF)]
    wst_b = [Buf() for _ in range(NSF)]
    wbf = [sb(f"wbf{i}", [128, 2048], BF16) for i in range(NST)]
    wbf_b = [Buf() for _ in range(NST)]
    wk = [0]

    def wload(src_ap, ncols=2048, scale=None, dst=None):
        i = wk[0] % NST
        f = wk[0] % NSF
        wk[0] += 1
        dma("sp", wst[f][:, 0:ncols], src_ap, writes=[wst_b[f]])
        ename = "pool" if (wk[0] % 2) else "dve"
        eng = nc.gpsimd if ename == "pool" else nc.vector
        if scale is not None:
            nk = ncols // 128
            o = wbf[i][:, 0:ncols].rearrange("p (c k) -> p c k", k=128)
            a = wst[f][:, 0:ncols].rearrange("p (c k) -> p c k", k=128)
            s = scale.unsqueeze(2).to_broadcast([128, nk, 128])
            op(ename, lambda: eng.tensor_tensor(out=o, in0=a, in1=s, op=ALU.mult),
               reads=[wst_b[f], cst_b], writes=[wbf_b[i]])
        elif dst is not None:
            op(ename, lambda: eng.tensor_copy(out=dst[0], in_=wst[f][:, 0:ncols]), reads=[wst_b[f]], writes=[dst[1]])
            return dst
        else:
            op(ename, lambda: eng.tensor_copy(out=wbf[i][:, 0:ncols], in_=wst[f][:, 0:ncols]),
               reads=[wst_b[f]], writes=[wbf_b[i]])
        return wbf[i], wbf_b[i]

    def dbg_dump(key, ap_sb, buf, dram_ap=None):
        if key in dbg_out:
            dma("sp", dbg_out[key][:] if dram_ap is None else dram_ap, ap_sb, reads=[buf])

    st_br = ExitStack()
    orecT = sb("orecT", [128, 8, 1024], BF16, st_br)
    orec_b = [Buf() for _ in range(8)]
    oattT = sb("oattT", [128, 8, 1024], BF16, st_br)
    oattT_b = Buf()
    mg_b = [Buf() for _ in range(16)]
    st_xo = ExitStack()
    st_xp = ExitStack()
    xnT_o = sb("xnT_o", [128, 16, 1024], BF16, st_xo)
    xnT_p = sb("xnT_p", [128, 16, 1024], BF16, st_xp)

    def xnT(c, t0, n):
        return xnT_p[:, c, t0:t0 + n] if t0 < 1024 else xnT_o[:, c, t0 - 1024:t0 - 1024 + n]

    def xnT8(c0, t0, n):
        return xnT_p[:, c0:c0 + 8, t0:t0 + n] if t0 < 1024 else xnT_o[:, c0:c0 + 8, t0 - 1024:t0 - 1024 + n]
    xnT_b = [Buf(f"xnT{t}") for t in range(16)]

    def norm_transpose(src_rows, dstT, dst_bufs, ntiles, stack, tag, src_is_sbuf=None):
        xin = [sb(f"{tag}xin{i}", [128, 2048], F32, stack) for i in range(2)]
        xin_b = [Buf(), Buf()]
        junk = sb(f"{tag}junk", [128, 2048], BF16, stack); junk_b = Buf()
        xs = [sb(f"{tag}xs{i}", [128, 2048], BF16, stack) for i in range(2)]
        xs_b = [Buf(), Buf()]
        st = sb(f"{tag}st", [128, 2, 4], F32, stack)
        st_b = [Buf(), Buf()]
        for tt in range(ntiles):
            s = tt % 2
            if src_is_sbuf is None:
                dma("sp", xin[s][:], src_rows(tt), writes=[xin_b[s]])
                xa, xb_ = xin[s][:], xin_b[s]
            else:
                xa, xb_ = src_is_sbuf(tt)
            op("act", lambda: nc.scalar.activation(out=junk[:], in_=xa, func=AF.Square, accum_out=st[:, s, 0:1]),
               reads=[xb_], writes=[junk_b, st_b[s]])
            op("dve", lambda: nc.vector.tensor_scalar(
                out=st[:, s, 1:2], in0=st[:, s, 0:1], scalar1=1.0 / 2048, scalar2=1e-6,
                op0=ALU.mult, op1=ALU.add), reads=[st_b[s]], writes=[st_b[s]])
            op("act", lambda: nc.scalar.activation(out=st[:, s, 2:3], in_=st[:, s, 1:2], func=AF.Sqrt),
               reads=[st_b[s]], writes=[st_b[s]])
            op("dve", lambda: nc.vector.reciprocal(out=st[:, s, 3:4], in_=st[:, s, 2:3]),
               reads=[st_b[s]], writes=[st_b[s]])
            op("dve", lambda: nc.vector.tensor_scalar(
                out=xs[s][:], in0=xa, scalar1=st[:, s, 3:4], scalar2=None, op0=ALU.mult),
               reads=[xb_, st_b[s]], writes=[xs_b[s]])
            for half in range(2):
                bk, bb = next_bank()
                bkh = bk[:].bitcast(BF16)
                for j in range(8):
                    c = half * 8 + j
                    op("pe", lambda: nc.tensor.transpose(
                        out=bkh[:, j * 128:(j + 1) * 128], in_=xs[s][:, c * 128:(c + 1) * 128],
                        identity=ident_h[:]), reads=[xs_b[s], ident_b], writes=[bb], signal=(j == 7))
                src = bkh.rearrange("p (c k) -> p c k", k=128)
                dst = dstT(half * 8, tt * 128, 128)
                if half == 0:
                    op("act", lambda: nc.scalar.copy(out=dst, in_=src), reads=[bb], writes=[dst_bufs[tt]])
                else:
                    op("dve", lambda: nc.vector.tensor_copy(out=dst, in_=src), reads=[bb], writes=[dst_bufs[tt]])

    with ExitStack() as st1:
        norm_transpose(lambda tt: xloc[tt * 128:(tt + 1) * 128, :], xnT8, xnT_b, 16, st1, "n1")
        fw.barrier()

    xnb = lambda ts: [xnT_b[t] for t in range(ts // 128, ts // 128 + 4)]
    def proj_fm(wb, wbb, srcT, src_bufs, nk, t0, t1, evac):
        for ts in range(t0, t1, 512):
            bk, bb = next_bank()
            rb = src_bufs(ts) if callable(src_bufs) else src_bufs
            for c in range(nk):
                op("pe", lambda: nc.tensor.matmul(bk[:], lhsT=wb[:, c * 128:(c + 1) * 128],
                                                  rhs=(srcT(c, ts, 512) if callable(srcT) else srcT[:, c, ts:ts + 512]), start=(c == 0), stop=(c == nk - 1)),
                   reads=[wbb] + rb, writes=[bb], signal=(c == nk - 1))
            evac(bk, bb, ts)

    GELU = AF.Gelu_apprx_tanh
    yrh = {0: wload(win_g[0], scale=gmix), 1: wload(win_g[1], scale=gmix)}
    for cc in range(8):
        if cc + 2 < 8:
            yrh[cc + 2] = wload(win_g[cc + 2], scale=gmix)
        wb, wbb = yrh.pop(cc)

        def ev_yr(bk, bb, ts, cc=cc):
            op("act", lambda: nc.scalar.activation(out=orecT[:, cc, ts - 1024:ts - 1024 + 512], in_=bk[:], func=GELU),
               reads=[bb], writes=[orec_b[cc]])
        proj_fm(wb, wbb, xnT, xnb, 16, 1024, 2048, ev_yr)

    with ExitStack() as st2:
        lw_f = sb("lw_f", [128, 2, 8, 128], F32, st2); lw_b = Buf()
        lw_h = sb("lw_h", [128, 2, 8, 128], BF16, st2)
        dma("sp", lw_f[:], lruw[:, :, :, :], writes=[lw_b])
        op("dve", lambda: nc.vector.tensor_copy(out=lw_h[:], in_=lw_f[:]), reads=[lw_b], writes=[lw_b])
        csp = sb("csp", [128, 4, 8], F32, st2); csp_b = Buf()
        op("act", lambda: nc.scalar.activation(out=csp[:, 0, :], in_=cst_sb[:, C_LAM:C_LAM + 8], func=AF.Exp, scale=-1.0),
           reads=[cst_b], writes=[csp_b])
        op("act", lambda: nc.scalar.activation(out=csp[:, 1, :], in_=csp[:, 0, :], func=AF.Ln, bias=1.0),
           reads=[csp_b], writes=[csp_b])
        op("dve", lambda: nc.vector.tensor_scalar(out=csp[:, 2, :], in0=csp[:, 1, :], scalar1=-8.0, scalar2=None, op0=ALU.mult),
           reads=[csp_b], writes=[csp_b])
        op("dve", lambda: nc.vector.tensor_scalar(out=csp[:, 3, :], in0=csp[:, 1, :], scalar1=-16.0, scalar2=None, op0=ALU.mult),
           reads=[csp_b], writes=[csp_b])
        T = 2048
        H = 1024
        xr = [sb(f"xr{i}", [128, T + 3], F32, st2) for i in range(2)]; xr_b = [Buf(), Buf()]
        for i in range(2):
            op("dve", lambda: nc.vector.memset(xr[i][:, 0:3], 0.0), writes=[xr_b[i]])
        tset = []
        for hf in range(2):
            d = {}
            for nm, dt_ in (("xc", F32), ("xch", BF16), ("rg", F32), ("ig", F32), ("sq", F32), ("hh", F32)):
                d[nm] = sb(f"{nm}{hf}", [128, H], dt_, st2)
                d[nm + "_b"] = Buf()
            tset.append(d)
        flag = cst_sb[:, C_FLAG:C_FLAG + 1]
        def xr_proj_gen(cc):
            wb, wbb = wload(win_g[8 + cc], scale=gmix)
            X, Xb = xr[cc % 2], xr_b[cc % 2]
            yield
            for ts in range(0, 2048, 512):
                bk, bb = next_bank()
                for c in range(16):
                    op("pe", lambda: nc.tensor.matmul(bk[:], lhsT=wb[:, c * 128:(c + 1) * 128], rhs=xnT(c, ts, 512),
                                                      start=(c == 0), stop=(c == 15)), reads=[wbb] + xnb(ts), writes=[bb], signal=(c == 15))
                op("act", lambda: nc.scalar.copy(out=X[:, 3 + ts:3 + ts + 512], in_=bk[:]), reads=[bb], writes=[Xb])
                yield

        def lru_chain(cc, hf):
            X, Xb = xr[cc % 2], xr_b[cc % 2]
            cw = lambda j: cst_sb[:, C_CONVW + cc * 4 + j:C_CONVW + cc * 4 + j + 1]
            cb = cst_sb[:, C_CONVB + cc:C_CONVB + cc + 1]
            d = tset[hf]
            o = hf * H
            xc, xc_b, xch, xch_b = d["xc"], d["xc_b"], d["xch"], d["xch_b"]
            rg, rg_b, ig, ig_b, sq, sq_b, hh, hh_b = d["rg"], d["rg_b"], d["ig"], d["ig_b"], d["sq"], d["sq_b"], d["hh"], d["hh_b"]
            aa, aa_b = rg, rg_b
            op("dve", lambda: nc.vector.tensor_scalar(out=xc[:], in0=X[:, o:o + H], scalar1=cw(0), scalar2=cb,
                                                      op0=ALU.mult, op1=ALU.add), reads=[Xb, cst_b], writes=[xc_b]); yield
            for j in range(1, 4):
                op("dve", lambda: nc.vector.scalar_tensor_tensor(out=xc[:], in0=X[:, o + j:o + j + H], scalar=cw(j), in1=xc[:],
                                                                 op0=ALU.mult, op1=ALU.add), reads=[Xb, xc_b, cst_b], writes=[xc_b]); yield
            op("act", lambda: nc.scalar.copy(out=xch[:], in_=xc[:]), reads=[xc_b], writes=[xch_b]); yield
            for gi, (dst, dstb, bcol) in enumerate(((rg, rg_b, C_BA), (ig, ig_b, C_BX))):
                for ts in range(0, H, 512):
                    bk, bb = next_bank()
                    op("pe", lambda: nc.tensor.matmul(bk[:], lhsT=lw_h[:, gi, cc, :], rhs=xch[:, ts:ts + 512], start=True, stop=True),
                       reads=[lw_b, xch_b], writes=[bb])
                    op("act", lambda: nc.scalar.activation(out=dst[:, ts:ts + 512], in_=bk[:], func=AF.Sigmoid,
                                                           bias=cst_sb[:, bcol + cc:bcol + cc + 1]), reads=[bb, cst_b], writes=[dstb])
                    yield
            op("act", lambda: nc.scalar.activation(out=sq[:], in_=rg[:], func=AF.Exp, scale=csp[:, 3, cc:cc + 1]),
               reads=[rg_b, csp_b], writes=[sq_b]); yield
            op("act", lambda: nc.scalar.activation(out=aa[:], in_=rg[:], func=AF.Exp, scale=csp[:, 2, cc:cc + 1]),
               reads=[rg_b, csp_b], writes=[aa_b]); yield
            op("act", lambda: nc.scalar.activation(out=sq[:], in_=sq[:], func=AF.Sqrt, scale=-1.0, bias=1.0),
               reads=[sq_b], writes=[sq_b]); yield
            if hf == 0:
                op("dve", lambda: nc.vector.scalar_tensor_tensor(out=ig[:], in0=ig[:], scalar=flag, in1=xc[:],
                                                                 op0=ALU.mult, op1=ALU.mult), reads=[ig_b, xc_b, cst_b], writes=[ig_b])
            else:
                op("dve", lambda: nc.vector.tensor_tensor(out=ig[:], in0=ig[:], in1=xc[:], op=ALU.mult),
                   reads=[ig_b, xc_b], writes=[ig_b])
            yield
            op("dve", lambda: nc.vector.tensor_tensor(out=sq[:], in0=sq[:], in1=ig[:], op=ALU.mult),
               reads=[sq_b, ig_b], writes=[sq_b]); yield
            if hf == 0:
                op("dve", lambda: nc.vector.tensor_tensor_scan(out=hh[:], data0=aa[:], data1=sq[:], initial=0.0,
                                                               op0=ALU.mult, op1=ALU.add), reads=[aa_b, sq_b], writes=[hh_b])
                yield
            else:
                h0, h0_b = tset[0]["hh"], tset[0]["hh_b"]
                op("dve", lambda: nc.vector.tensor_tensor_scan(out=hh[:], data0=aa[:], data1=sq[:], initial=h0[:, H - 1:H],
                                                               op0=ALU.mult, op1=ALU.add), reads=[aa_b, sq_b, h0_b], writes=[hh_b])
                yield
                op("dve", lambda: nc.vector.tensor_tensor(out=orecT[:, cc, :], in0=orecT[:, cc, :], in1=hh[:], op=ALU.mult),
                   reads=[hh_b, orec_b[cc]], writes=[orec_b[cc]])
                yield

        tasks = []
        for cc in range(8):
            dp = []
            if cc >= 1:
                dp.append(("P", cc - 1))
            if cc >= 2:
                dp += [("C", cc - 2, 0), ("C", cc - 2, 1)]
            tasks.append((("P", cc), (lambda cc=cc: xr_proj_gen(cc)), dp))
            d0 = [("P", cc)] + ([("C", cc - 1, 0)] if cc >= 1 else [])
            d1 = d0 + ([("C", cc - 1, 1)] if cc >= 1 else [])
            tasks.append((("C", cc, 0), (lambda cc=cc: lru_chain(cc, 0)), d0))
            tasks.append((("C", cc, 1), (lambda cc=cc: lru_chain(cc, 1)), d1))
        done_t = set()
        active = []
        pending = list(tasks)
        while pending or active:
            for t in list(pending):
                if all(dd in done_t for dd in t[2]):
                    active.append((t[0], t[1]()))
                    pending.remove(t)
            assert active, "scheduler stuck"
            for item in list(active):
                try:
                    next(item[1])
                except StopIteration:
                    active.remove(item)
                    done_t.add(item[0])
        fw.barrier()

    if "orecT" in dbg_out:
        with ExitStack() as sd:
            tmp = sb("dbgtmp", [128, 8, 1024], F32, sd); tb = Buf()
            op("dve", lambda: nc.vector.tensor_copy(out=tmp[:], in_=orecT[:]), reads=orec_b, writes=[tb])
            dma("sp", dbg_out["orecT"][:, :, :], tmp[:], reads=[tb])
            fw.barrier()

    STAGE = DEBUG.get("stage", 99)

    def finish_zero():
        with ExitStack() as sd:
            z = sb("zz", [128, 2048], F32, sd); zb = Buf()
            op("dve", lambda: nc.vector.memset(z[:], 0.0), writes=[zb])
            for t in range(8):
                dma("sp", y[t * 128:(t + 1) * 128, :], z[:], reads=[zb])
            fw.barrier()
        st_xp.close(); st_xo.close(); st_br.close()
        es.close()
        return nc

    if STAGE <= 2:
        return finish_zero()

    b31 = cst_sb[:, C_B31:C_B31 + 8]
    with ExitStack() as st3:
        oatt2 = [sb(f"oatt{i}", [128, 8, 128], BF16, st3) for i in range(2)]; oatt_b2 = [Buf(), Buf()]
        ownb2 = [sb(f"ownb{i}", [128, 2, 256], F32, st3) for i in range(2)]; ownb_b2 = [Buf(), Buf()]
        prevb2 = [sb(f"prevb{i}", [128, 256], F32, st3) for i in range(2)]; prevb_b2 = [Buf(), Buf()]
        blkv = sb("blkv", [128, 8, 8], F32, st3); blkv_b = Buf()
        dma("sp", blkv[:], blkvalid_d[:, :, :], writes=[blkv_b])
        qT = [sb(f"qT{i}", [128, 1024], BF16, st3) for i in range(2)]; qT_b = [Buf(), Buf()]
        kT = [sb(f"kT{i}", [128, 2056], BF16, st3) for i in range(2)]; kT_b = [Buf(), Buf()]
        vE = [sb(f"vE{i}", [128, 16, 130], BF16, st3) for i in range(2)]; vE_b = [Buf(), Buf()]
        km = sb("km", [128, 2, 8], F32, st3); km_b = [Buf(), Buf()]
        NL = 3
        S_sb = [sb(f"S{i}", [128, 2048], F32, st3) for i in range(NL)]; S_b = [Buf() for _ in range(NL)]
        P_sb = [sb(f"P{i}", [128, 2048], BF16, st3) for i in range(NL)]; P_b = [Buf() for _ in range(NL)]
        PT = [sb(f"PT{i}", [128, 16, 128], BF16, st3) for i in range(NL)]; PT_b = [Buf() for _ in range(NL)]
        sm = [sb(f"sm{i}", [128, 64], F32, st3) for i in range(NL)]; sm_b = [Buf() for _ in range(NL)]
        for i in range(2):
            op("dve", lambda: nc.vector.memset(vE[i][:, :, 128:130], 1.0), writes=[vE_b[i]])
        def run_rr(gens):
            alive = [True] * len(gens)
            while any(alive):
                for gi_ in range(len(gens)):
                    if alive[gi_]:
                        try:
                            next(gens[gi_])
                        except StopIteration:
                            alive[gi_] = False

        free_banks = list(range(8))

        def acquire(n):
            while len(free_banks) < n:
                yield None
            got = [free_banks.pop(0) for _ in range(n)]
            return [(banks[i], bank_bufs[i], i) for i in got]

        def release(lst):
            for (_, _, i) in lst:
                free_banks.append(i)

        def proj_head(h):
            s = h % 2
            ownb, ownb_b, prevb, prevb_b = ownb2[s], ownb_b2[s], prevb2[s], prevb_b2[s]
            dma("sp", ownb[:], ownbias_d[:, h, :, :], writes=[ownb_b])
            dma("sp", prevb[:], prevbias_d[:, h, :], writes=[prevb_b])
            wb, wbb = wload(win_g[16 + 3 * h], scale=gmix)
            yield
            for ts in (1024, 1536):
                L = yield from acquire(1)
                bk, bb, _ = L[0]
                for c in range(16):
                    op("pe", lambda: nc.tensor.matmul(bk[:], lhsT=wb[:, c * 128:(c + 1) * 128], rhs=xnT(c, ts, 512),
                                                      start=(c == 0), stop=(c == 15)), reads=[wbb] + xnb(ts), writes=[bb], signal=(c == 15))
                    if c % 4 == 3:
                        yield
                op("act", lambda: nc.scalar.copy(out=qT[s][:, ts - 1024:ts - 512], in_=bk[:]), reads=[bb], writes=[qT_b[s]])
                release(L)
                yield
            wb, wbb = wload(win_g[16 + 3 * h + 1], scale=gmix)
            yield
            for ts in range(0, 2048, 512):
                L = yield from acquire(1)
                bk, bb, _ = L[0]
                for c in range(16):
                    op("pe", lambda: nc.tensor.matmul(bk[:], lhsT=wb[:, c * 128:(c + 1) * 128], rhs=xnT(c, ts, 512),
                                                      start=(c == 0), stop=(c == 15)), reads=[wbb] + xnb(ts), writes=[bb], signal=(c == 15))
                    if c % 4 == 3:
                        yield
                op("act", lambda: nc.scalar.copy(out=kT[s][:, ts:ts + 512], in_=bk[:]), reads=[bb], writes=[kT_b[s]])
                release(L)
                yield
            op("dve", lambda: nc.vector.tensor_reduce(out=km[:, s, :], in_=kT[s][:, 0:2048].rearrange("p (b k) -> p b k", k=256),
                                                      axis=AX.X, op=ALU.add), reads=[kT_b[s]], writes=[km_b[s]])
            yield
            op("dve", lambda: nc.vector.tensor_scalar(out=kT[s][:, 2048:2056], in0=km[:, s, :], scalar1=1.0 / 256, scalar2=None,
                                                      op0=ALU.mult), reads=[km_b[s]], writes=[kT_b[s]])
            wb, wbb = wload(win_g[16 + 3 * h + 2], scale=gmix)
            yield
            for tg in range(4):
                L = yield from acquire(1)
                bk, bb, _ = L[0]
                for j in range(4):
                    tile = tg * 4 + j
                    for c in range(16):
                        op("pe", lambda: nc.tensor.matmul(bk[:, j * 128:(j + 1) * 128], lhsT=xnT(c, tile * 128, 128),
                                                          rhs=wb[:, c * 128:(c + 1) * 128], start=(c == 0), stop=(c == 15)),
                           reads=[wbb, xnT_b[tile]], writes=[bb], signal=(c == 15 and j == 3))
                        if c % 4 == 3:
                            yield
                src = bk[:].rearrange("p (a k) -> p a k", k=128)
                dst = vE[s][:, tg * 4:(tg + 1) * 4, 0:128]
                op("act", lambda: nc.scalar.copy(out=dst, in_=src), reads=[bb], writes=[vE_b[s]])
                release(L)
                yield

        def attn_lane(h, u):
            s = h % 2
            ownb, ownb_b, prevb, prevb_b = ownb2[s], ownb_b2[s], prevb2[s], prevb_b2[s]
            for qt in range(u, 8, NL):
                ob = 4 + qt // 2
                par = qt % 2
                nk = 256 * (ob + 1)
                lq = qT[s][:, qt * 128:(qt + 1) * 128]
                nsb = (nk + 511) // 512
                L1 = yield from acquire(nsb + 1)
                bg, bgb, _ = L1[0]
                op("pe", lambda: nc.tensor.matmul(bg[:, 0:8], lhsT=lq, rhs=kT[s][:, 2048:2056], start=True, stop=True),
                   reads=[qT_b[s], kT_b[s]], writes=[bgb])
                sbanks = []
                for kr in range(0, nk, 512):
                    w = min(512, nk - kr)
                    bk, bb, _ = L1[1 + kr // 512]
                    op("pe", lambda: nc.tensor.matmul(bk[:, 0:w], lhsT=lq, rhs=kT[s][:, kr:kr + w], start=True, stop=True),
                       reads=[qT_b[s], kT_b[s]], writes=[bb])
                    sbanks.append((bk, bb))
                yield
                m, mb = sm[u], sm_b[u]
                gm, top8, thr, selm, madd, madd2 = m[:, 0:8], m[:, 8:16], m[:, 16:17], m[:, 24:32], m[:, 32:40], m[:, 40:48]
                op("dve", lambda: nc.vector.tensor_tensor(out=gm, in0=bg[:, 0:8], in1=blkv[:, qt, :], op=ALU.add),
                   reads=[bgb, blkv_b], writes=[mb]); yield
                op("dve", lambda: nc.vector.max(out=top8, in_=gm), reads=[mb], writes=[mb]); yield
                op("dve", lambda: nc.vector.tensor_scalar(out=thr, in0=top8[:, 2:3], scalar1=-1e29, scalar2=None, op0=ALU.max),
                   reads=[mb], writes=[mb]); yield
                op("dve", lambda: nc.vector.tensor_scalar(out=selm, in0=gm, scalar1=thr, scalar2=None, op0=ALU.is_ge),
                   reads=[mb], writes=[mb]); yield
                op("dve", lambda: nc.vector.tensor_scalar(out=madd, in0=selm, scalar1=-1.0, scalar2=1e30, op0=ALU.add, op1=ALU.mult),
                   reads=[mb], writes=[mb]); yield
                op("dve", lambda: nc.vector.tensor_scalar(out=madd2, in0=madd, scalar1=b31[:, h:h + 1], scalar2=None, op0=ALU.add),
                   reads=[mb, cst_b], writes=[mb]); yield
                S, Sb = S_sb[u], S_b[u]
                for blk in range(ob + 1):
                    bk, bb = sbanks[blk // 2]
                    src = bk[:, (blk % 2) * 256:(blk % 2) * 256 + 256]
                    dstS = S[:, blk * 256:(blk + 1) * 256]
                    if blk < ob:
                        near = (blk == ob - 1 and par == 0)
                        ma = madd if near else madd2
                        if near:
                            op("dve", lambda: nc.vector.tensor_scalar(out=dstS, in0=src, scalar1=SCALE, scalar2=ma[:, blk:blk + 1],
                                                                      op0=ALU.mult, op1=ALU.add), reads=[bb, mb], writes=[Sb])
                        else:
                            op("act", lambda: nc.scalar.activation(out=dstS, in_=src, func=AF.Identity, scale=SCALE, bias=ma[:, blk:blk + 1]),
                               reads=[bb, mb], writes=[Sb])
                        if near:
                            op("dve", lambda: nc.vector.tensor_tensor(out=dstS, in0=dstS, in1=prevb[:], op=ALU.add),
                               reads=[Sb, prevb_b], writes=[Sb])
                    else:
                        op("dve", lambda: nc.vector.scalar_tensor_tensor(out=dstS, in0=src, scalar=SCALE, in1=ownb[:, par, :],
                                                                         op0=ALU.mult, op1=ALU.add), reads=[bb, ownb_b], writes=[Sb])
                    yield
                release(L1)
                op("dve", lambda: nc.vector.tensor_reduce(out=m[:, 48:49], in_=S[:, 0:nk], axis=AX.X, op=ALU.max),
                   reads=[Sb], writes=[mb]); yield
                op("dve", lambda: nc.vector.tensor_scalar(out=m[:, 49:50], in0=m[:, 48:49], scalar1=-1.0, scalar2=None, op0=ALU.mult),
                   reads=[mb], writes=[mb]); yield
                op("act", lambda: nc.scalar.activation(out=P_sb[u][:, 0:nk], in_=S[:, 0:nk], func=AF.Exp, bias=m[:, 49:50], scale=1.0),
                   reads=[Sb, mb], writes=[P_b[u]]); yield
                nkt = nk // 128
                for k0 in range(0, nkt, 8):
                    L2 = yield from acquire(1)
                    bk, bb, _ = L2[0]
                    bkh = bk[:].bitcast(BF16)
                    n8 = min(8, nkt - k0)
                    for j in range(n8):
                        kt = k0 + j
                        op("pe", lambda: nc.tensor.transpose(out=bkh[:, j * 128:(j + 1) * 128], in_=P_sb[u][:, kt * 128:(kt + 1) * 128],
                                                             identity=ident_h[:]), reads=[P_b[u], ident_b], writes=[bb], signal=(j == n8 - 1))
                    src = bkh[:, 0:n8 * 128].rearrange("p (c k) -> p c k", k=128)
                    dst = PT[u][:, k0:k0 + n8, :]
                    op("act", lambda: nc.scalar.copy(out=dst, in_=src), reads=[bb], writes=[PT_b[u]])
                    release(L2)
                    yield
                L3 = yield from acquire(1)
                bo, bob, _ = L3[0]
                for kt in range(nkt):
                    op("pe", lambda: nc.tensor.matmul(bo[:, 0:129], lhsT=PT[u][:, kt, :], rhs=vE[s][:, kt, 0:129],
                                                      start=(kt == 0), stop=(kt == nkt - 1)),
                       reads=[PT_b[u], vE_b[s]], writes=[bob], signal=(kt == nkt - 1))
                yield
                op("dve", lambda: nc.vector.reciprocal(out=m[:, 50:51], in_=bo[:, 128:129]), reads=[bob], writes=[mb]); yield
                op("dve", lambda: nc.vector.tensor_scalar(out=oatt2[s][:, qt, :], in0=bo[:, 0:128], scalar1=m[:, 50:51],
                                                          scalar2=None, op0=ALU.mult), reads=[bob, mb], writes=[oatt_b2[s]])
                release(L3)
                yield

        def oatt_T(h):
            s_ = h % 2
            L = yield from acquire(1)
            bk, bb, _ = L[0]
            bkh = bk[:].bitcast(BF16)
            for qt in range(8):
                op("pe", lambda: nc.tensor.transpose(out=bkh[:, qt * 128:(qt + 1) * 128], in_=oatt2[s_][:, qt, :],
                                                     identity=ident_h[:]), reads=[oatt_b2[s_], ident_b], writes=[bb], signal=(qt == 7))
            op("act", lambda: nc.scalar.copy(out=oattT[:, h, :], in_=bkh), reads=[bb], writes=[oattT_b])
            release(L)
            yield

        run_rr([proj_head(0)])
        for h in range(8):
            gens = [attn_lane(h, u) for u in range(NL)]
            if h < 7:
                gens.append(proj_head(h + 1))
            if h >= 1:
                gens.append(oatt_T(h - 1))
            run_rr(gens)
        run_rr([oatt_T(7)])
        fw.barrier()

    if "oattT" in dbg_out:
        with ExitStack() as sd:
            tmp = sb("dbgtmp2", [128, 8, 1024], F32, sd); tb = Buf()
            op("dve", lambda: nc.vector.tensor_copy(out=tmp[:], in_=oattT[:]), reads=[oattT_b], writes=[tb])
            dma("sp", dbg_out["oattT"][:, :, :], tmp[:], reads=[tb])
            fw.barrier()
    if STAGE <= 3:
        return finish_zero()

    st_xp.close()
    st_mg = ExitStack()
    mergedT = sb("mergedT", [128, 16, 1024], BF16, st_mg)
    with ExitStack() as st4:
        sg = [sb(f"sg{i}", [128, 1024], F32, st4) for i in range(2)]; sg_b = [Buf(), Buf()]
        tm = sb("tm", [128, 1024], F32, st4); tm_b = Buf()
        sgB = [sb(f"sgB{i}", [128, 1024], F32, st4) for i in range(2)]; sgB_b = [Buf(), Buf()]
        tmB = sb("tmB", [128, 1024], F32, st4); tmB_b = Buf()
        SG2 = [(sg, sg_b), (sgB, sgB_b)]
        TM2 = [(tm, tm_b), (tmB, tmB_b)]
        items = []
        for j in range(16):
            for n in range(2):
                items.append(("g", j, n))
            for n in range(2):
                items.append(("b", j, n))

        def load_item(it_):
            kind, j, n = it_
            if kind == "g":
                return wload(win_g[40 + n * 16 + j], scale=gmix)
            return wload(wbr_g[n * 16 + j], ncols=1024)

        handles = {}
        for i0 in range(min(2, len(items))):
            handles[i0] = load_item(items[i0])
        for i_, it_ in enumerate(items):
            if i_ + 2 < len(items):
                handles[i_ + 2] = load_item(items[i_ + 2])
            wb, wbb = handles.pop(i_)
            kind, j, n = it_
            sgs, sgs_b = SG2[j % 2]
            tmj, tmj_b = TM2[j % 2]
            if kind == "g":
                def ev_g(bk, bb, ts, n=n, sgs=sgs, sgs_b=sgs_b):
                    op("act", lambda: nc.scalar.activation(out=sgs[n][:, ts - 1024:ts - 512], in_=bk[:], func=AF.Sigmoid),
                       reads=[bb], writes=[sgs_b[n]])
                proj_fm(wb, wbb, xnT, xnb, 16, 1024, 2048, ev_g)
            else:
                srcT, srcb = (oattT, [oattT_b]) if n == 0 else (orecT, orec_b)

                def ev_b(bk, bb, ts, n=n, j=j, sgs=sgs, sgs_b=sgs_b, tmj=tmj, tmj_b=tmj_b):
                    if n == 0:
                        op("dve", lambda: nc.vector.tensor_tensor(out=tmj[:, ts:ts + 512], in0=bk[:], in1=sgs[0][:, ts:ts + 512], op=ALU.mult),
                           reads=[bb, sgs_b[0]], writes=[tmj_b])
                    else:
                        op("dve", lambda: nc.vector.tensor_tensor(out=sgs[1][:, ts:ts + 512], in0=bk[:], in1=sgs[1][:, ts:ts + 512], op=ALU.mult),
                           reads=[bb, sgs_b[1]], writes=[sgs_b[1]])
                        op("dve", lambda: nc.vector.tensor_tensor(out=mergedT[:, j, ts:ts + 512], in0=tmj[:, ts:ts + 512],
                                                                  in1=sgs[1][:, ts:ts + 512], op=ALU.add),
                           reads=[tmj_b, sgs_b[1]], writes=[mg_b[j]])
                proj_fm(wb, wbb, srcT, srcb, 8, 0, 1024, ev_b)
        fw.barrier()
    with ExitStack() as st5:
        xsl = [sb(f"xsl{i}", [128, 4, 128], F32, st5) for i in range(4)]; xsl_b = [Buf() for _ in range(4)]
        k5 = 0
        woh = {0: wload(wout_g[0]), 1: wload(wout_g[1])}
        for g in range(16):
            if g + 2 < 16:
                woh[g + 2] = wload(wout_g[g + 2])
            wb, wbb = woh.pop(g)
            for hb in range(2):
                u = k5 % 4
                k5 += 1
                dview = lambda t: t[1024 * 0 + hb * 512:hb * 512 + 512, g * 128:(g + 1) * 128].rearrange("(k p) c -> p k c", p=128)
                dma("sp", xsl[u][:], xloc[1024 + hb * 512:1024 + hb * 512 + 512, g * 128:(g + 1) * 128].rearrange("(k p) c -> p k c", p=128),
                    writes=[xsl_b[u]])
                bk, bb = next_bank()
                for k in range(4):
                    tt = hb * 4 + k
                    for j in range(16):
                        op("pe", lambda: nc.tensor.matmul(bk[:, k * 128:(k + 1) * 128], lhsT=mergedT[:, j, tt * 128:(tt + 1) * 128],
                                                          rhs=wb[:, j * 128:(j + 1) * 128], start=(j == 0), stop=(j == 15)),
                           reads=[wbb, mg_b[j]], writes=[bb], signal=(j == 15 and k == 3))
                op("dve", lambda: nc.vector.tensor_tensor(out=xsl[u][:], in0=bk[:].rearrange("p (a k) -> p a k", k=128), in1=xsl[u][:], op=ALU.add),
                   reads=[bb, xsl_b[u]], writes=[xsl_b[u]])
                dma("sp", dview(x2s), xsl[u][:], reads=[xsl_b[u]], writes=[x2s_b])
        fw.barrier()
    st_mg.close(); st_xo.close(); st_br.close()
    if STAGE <= 4:
        with ExitStack() as sd:
            xt = sb("xt4", [128, 2048], F32, sd); xtb = Buf()
            for t in range(8):
                dma("sp", xt[:], x2s[t * 128:(t + 1) * 128, :], reads=[x2s_b], writes=[xtb])
                dma("sp", y[t * 128:(t + 1) * 128, :], xt[:], reads=[xtb])
            fw.barrier()
        es.close()
        return nc

    st_p = ExitStack()
    xn2T = sb("xn2T", [128, 16, 1024], BF16, st_p)
    xn2_b = [Buf() for _ in range(8)]
    IGG = sb("IGG", [128, 3, 8, 128], F32, st_p)
    igg_b = [Buf() for _ in range(8)]
    iota_f = sb("iota_f", [128, 128], F32, st_p); iota_b = Buf()
    dma("sp", iota_f[:], iota_d[:, :], writes=[iota_b])
    with ExitStack() as stn:
        norm_transpose(lambda tt: x2s[tt * 128:(tt + 1) * 128, :], lambda c0, t0, n: xn2T[:, c0:c0 + 8, t0:t0 + n], xn2_b, 8, stn, "n2")
        fw.barrier()
    xn2b = lambda ts: [xn2_b[t] for t in range(ts // 128, ts // 128 + 4)]
    with ExitStack() as sts:
        s_all = sb("s_all", [128, 8, 16, 128], F32, sts)
        sall_b = [Buf() for _ in range(8)]
        kst = sb("kst", [128, 16, 128], F32, sts); kst_b = Buf()
        kbf = sb("kbf", [128, 16, 128], BF16, sts)
        dma("sp", kst[:], keysT[:, :, :], writes=[kst_b])
        op("dve", lambda: nc.vector.tensor_copy(out=kbf[:], in_=kst[:]), reads=[kst_b], writes=[kst_b])
        qTp = [sb(f"qTp{i}", [128, 1024], BF16, sts) for i in range(2)]; qTp_b = [Buf(), Buf()]
        def q_scores(pair):
            s = pair % 2
            for hb in range(2):
                bk, bb = next_bank()
                for k in range(4):
                    tt = hb * 4 + k
                    op("pe", lambda: nc.tensor.matmul(bk[:, k * 128:(k + 1) * 128], lhsT=qTp[s][:, tt * 128:(tt + 1) * 128],
                                                      rhs=kbf[:, pair, :], start=True, stop=True),
                       reads=[qTp_b[s], kst_b], writes=[bb], signal=(k == 3))
                dst = s_all[:, hb * 4:hb * 4 + 4, pair, :]
                op("dve", lambda: nc.vector.tensor_copy(out=dst, in_=bk[:].rearrange("p (a k) -> p a k", k=128)),
                   reads=[bb], writes=sall_b[hb * 4:hb * 4 + 4])

        wqh = {0: wload(wq_g[0], scale=gffn), 1: wload(wq_g[1], scale=gffn)}
        for pair in range(16):
            s = pair % 2
            if pair + 2 < 16:
                wqh[pair + 2] = wload(wq_g[pair + 2], scale=gffn)
            wb, wbb = wqh.pop(pair)

            def ev_qp(bk, bb, ts, s=s):
                op("act", lambda: nc.scalar.copy(out=qTp[s][:, ts:ts + 512], in_=bk[:]), reads=[bb], writes=[qTp_b[s]])
            proj_fm(wb, wbb, xn2T, xn2b, 16, 0, 1024, ev_qp)
            if pair >= 1:
                q_scores(pair - 1)
        q_scores(15)
        it16 = sb("it16", [128, 16], F32, sts)
        op("dve", lambda: nc.vector.tensor_scalar(out=it16[:], in0=iota_f[:, 0:16], scalar1=16.0, scalar2=None, op0=ALU.mult),
           reads=[iota_b], writes=[iota_b])
        V = nc.vector

        def mk_scratch(i):
            d = {}
            d["sv"] = sb(f"sv{i}", [128, 16, 16], F32, sts); d["sv_b"] = Buf()
            d["si"] = sb(f"si{i}", [128, 16, 16], U32, sts)
            d["sif"] = sb(f"sif{i}", [128, 16, 16], F32, sts)
            d["sw"] = sb(f"sw{i}", [128, 128], F32, sts); d["sw_b"] = Buf()
            d["cand"] = sb(f"cand{i}", [128, 8, 16, 16], F32, sts); d["cand_b"] = Buf()
            d["cw"] = sb(f"cw{i}", [128, 256], F32, sts); d["cw_b"] = Buf()
            d["cv"] = sb(f"cv{i}", [128, 8, 16], F32, sts); d["cv_b"] = Buf()
            d["ci"] = sb(f"ci{i}", [128, 8, 16], U32, sts)
            d["cif"] = sb(f"cif{i}", [128, 8, 16], F32, sts)
            d["kf"] = sb(f"kf{i}", [128, 2, 8, 16], F32, sts); d["kf_b"] = Buf()
            d["ce"] = sb(f"ce{i}", [128, 8, 16], F32, sts); d["ce_b"] = Buf()
            d["zz"] = sb(f"zz5{i}", [128, 2, 8], F32, sts); d["zz_b"] = Buf()
            return d
        scr = [mk_scratch(0), mk_scratch(1)]

        def topk_tile(tt, d):
            sv, sv_b, si, sif, sw, sw_b = d["sv"], d["sv_b"], d["si"], d["sif"], d["sw"], d["sw_b"]
            cand, cand_b, cw, cw_b, cv, cv_b, ci, cif = d["cand"], d["cand_b"], d["cw"], d["cw_b"], d["cv"], d["cv_b"], d["ci"], d["cif"]
            kf, kf_b, ce, ce_b, zz, zz_b = d["kf"], d["kf_b"], d["ce"], d["ce_b"], d["zz"], d["zz_b"]
            eq, eq_b = cand, cand_b
            for p in range(16):
                src = s_all[:, tt, p, :]
                op("dve", lambda: V.max(out=sv[:, p, 0:8], in_=src), reads=[sall_b[tt]], writes=[sv_b]); yield
                op("dve", lambda: V.max_index(out=si[:, p, 0:8], in_max=sv[:, p, 0:8], in_values=src), reads=[sall_b[tt], sv_b], writes=[sv_b]); yield
                op("dve", lambda: V.match_replace(out=sw[:], in_to_replace=sv[:, p, 0:8], in_values=src, imm_value=NEG),
                   reads=[sall_b[tt], sv_b], writes=[sw_b]); yield
                op("dve", lambda: V.max(out=sv[:, p, 8:16], in_=sw[:]), reads=[sw_b], writes=[sv_b]); yield
                op("dve", lambda: V.max_index(out=si[:, p, 8:16], in_max=sv[:, p, 8:16], in_values=sw[:]), reads=[sw_b, sv_b], writes=[sv_b]); yield
            sv4 = sv[:].rearrange("p (h c) k -> p h c k", c=2)
            op("dve", lambda: V.tensor_tensor(out=cand[:], in0=sv4[:, :, 0, :].unsqueeze(3).to_broadcast([128, 8, 16, 16]),
                                              in1=sv4[:, :, 1, :].unsqueeze(2).to_broadcast([128, 8, 16, 16]), op=ALU.add),
               reads=[sv_b], writes=[cand_b]); yield
            for h in range(8):
                src = cand[:, h, :, :].rearrange("p a b -> p (a b)")
                op("dve", lambda: V.max(out=cv[:, h, 0:8], in_=src), reads=[cand_b], writes=[cv_b]); yield
                op("dve", lambda: V.max_index(out=ci[:, h, 0:8], in_max=cv[:, h, 0:8], in_values=src), reads=[cand_b, cv_b], writes=[cv_b]); yield
                op("dve", lambda: V.match_replace(out=cw[:], in_to_replace=cv[:, h, 0:8], in_values=src, imm_value=NEG),
                   reads=[cand_b, cv_b], writes=[cw_b]); yield
                op("dve", lambda: V.max(out=cv[:, h, 8:16], in_=cw[:]), reads=[cw_b], writes=[cv_b]); yield
                op("dve", lambda: V.max_index(out=ci[:, h, 8:16], in_max=cv[:, h, 8:16], in_values=cw[:]), reads=[cw_b, cv_b], writes=[cv_b]); yield
            op("dve", lambda: V.tensor_copy(out=cif[:], in_=ci[:]), reads=[cv_b], writes=[kf_b]); yield
            op("dve", lambda: V.tensor_tensor(out=eq[:], in0=cif[:].unsqueeze(3).to_broadcast([128, 8, 16, 16]),
                                              in1=it16[:].unsqueeze(1).unsqueeze(1).to_broadcast([128, 8, 16, 16]), op=ALU.is_ge),
               reads=[kf_b, cv_b], writes=[eq_b]); yield
            op("dve", lambda: V.tensor_reduce(out=kf[:, 0, :, :], in_=eq[:], axis=AX.X, op=ALU.add), reads=[eq_b], writes=[kf_b]); yield
            op("dve", lambda: V.tensor_scalar(out=kf[:, 0, :, :], in0=kf[:, 0, :, :], scalar1=-1.0, scalar2=None, op0=ALU.add),
               reads=[kf_b], writes=[kf_b]); yield
            op("dve", lambda: V.scalar_tensor_tensor(out=kf[:, 1, :, :], in0=kf[:, 0, :, :], scalar=-16.0, in1=cif[:], op0=ALU.mult, op1=ALU.add),
               reads=[kf_b], writes=[kf_b]); yield
            op("dve", lambda: V.tensor_copy(out=sif[:], in_=si[:]), reads=[sv_b], writes=[sv_b]); yield
            sif4 = sif[:].rearrange("p (h c) k -> p h c k", c=2)
            for c2 in range(2):
                op("dve", lambda: V.tensor_tensor(out=eq[:], in0=iota_f[:, 0:16].unsqueeze(1).unsqueeze(1).to_broadcast([128, 8, 16, 16]),
                                                  in1=kf[:, c2, :, :].unsqueeze(3).to_broadcast([128, 8, 16, 16]), op=ALU.is_equal),
                   reads=[kf_b, iota_b], writes=[eq_b]); yield
                op("dve", lambda: V.tensor_tensor(out=eq[:], in0=eq[:], in1=sif4[:, :, c2, :].unsqueeze(2).to_broadcast([128, 8, 16, 16]), op=ALU.mult),
                   reads=[eq_b, sv_b], writes=[eq_b]); yield
                op("dve", lambda: V.tensor_reduce(out=IGG[:, c2, tt, :].rearrange("p (h k) -> p h k", k=16), in_=eq[:], axis=AX.X, op=ALU.add),
                   reads=[eq_b], writes=[igg_b[tt]]); yield
            op("dve", lambda: V.tensor_tensor(out=ce[:], in0=cv[:], in1=cv[:, :, 0:1].to_broadcast([128, 8, 16]), op=ALU.subtract),
               reads=[cv_b], writes=[ce_b]); yield
            op("act", lambda: nc.scalar.activation(out=ce[:], in_=ce[:], func=AF.Exp), reads=[ce_b], writes=[ce_b]); yield
            op("dve", lambda: V.tensor_reduce(out=zz[:, 0, :], in_=ce[:], axis=AX.X, op=ALU.add), reads=[ce_b], writes=[zz_b]); yield
            op("dve", lambda: V.reciprocal(out=zz[:, 1, :], in_=zz[:, 0, :]), reads=[zz_b], writes=[zz_b]); yield
            op("dve", lambda: V.tensor_tensor(out=IGG[:, 2, tt, :].rearrange("p (h k) -> p h k", k=16), in0=ce[:],
                                              in1=zz[:, 1, :].unsqueeze(2).to_broadcast([128, 8, 16]), op=ALU.mult),
               reads=[ce_b, zz_b], writes=[igg_b[tt]]); yield

        for t2 in range(0, 8, 2):
            gens = [topk_tile(t2, scr[0]), topk_tile(t2 + 1, scr[1])]
            alive = [True, True]
            while any(alive):
                for gi_ in range(2):
                    if alive[gi_]:
                        try:
                            next(gens[gi_])
                        except StopIteration:
                            alive[gi_] = False
        fw.barrier()
    if "igg" in dbg_out:
        dma("sp", dbg_out["igg"][:, :, :, :], IGG[:], reads=igg_b)
        fw.barrier()
    with ExitStack() as stg:
        NQ = 32
        Aoh2 = [sb(f"Aoh{i}", [128, 128, NQ], BF16, stg) for i in range(2)]; A_b2 = [Buf(), Buf()]
        Boh2 = [sb(f"Boh{i}", [128, 128, NQ], BF16, stg) for i in range(2)]; B_b2 = [Buf(), Buf()]
        Gs2 = [sb(f"Gs{i}", [128, 128, 128], BF16, stg) for i in range(2)]; Gs_b2 = [Buf(), Buf()]
        iT2 = [sb(f"iT{i}", [128, 3, 128], BF16, stg) for i in range(2)]; iT_b2 = [Buf(), Buf()]
        iocol = sb("iocol", [128, 128, NQ], BF16, stg)
        op("dve", lambda: nc.vector.tensor_copy(out=iocol[:], in_=iota_f[:].unsqueeze(2).to_broadcast([128, 128, NQ])),
           reads=[iota_b], writes=[iota_b])
        qk = 0
        for tt in range(8):
            iT, iT_b = iT2[tt % 2], iT_b2[tt % 2]
            Gs, Gs_b = Gs2[tt % 2], Gs_b2[tt % 2]
            for q3 in range(3):
                bk, bb = next_bank()
                op("pe", lambda: nc.tensor.transpose(out=bk[:, 0:128], in_=IGG[:, q3, tt, :], identity=ident_f[:]),
                   reads=[igg_b[tt], ident_b], writes=[bb])
                op("act", lambda: nc.scalar.copy(out=iT[:, q3, :], in_=bk[:, 0:128]), reads=[bb], writes=[iT_b])
            for o in range(0, 128, NQ):
                u = qk % 2
                qk += 1
                Aoh, A_b, Boh, B_b = Aoh2[u], A_b2[u], Boh2[u], B_b2[u]
                bc = lambda q3: iT[:, q3, o:o + NQ].unsqueeze(1).to_broadcast([128, 128, NQ])
                op("dve", lambda: nc.vector.tensor_tensor(out=Aoh[:], in0=iocol[:], in1=bc(0), op=ALU.is_equal),
                   reads=[iT_b, iota_b], writes=[A_b])
                op("dve", lambda: nc.vector.tensor_tensor(out=Boh[:], in0=iocol[:], in1=bc(1), op=ALU.is_equal),
                   reads=[iT_b, iota_b], writes=[B_b])
                op("dve", lambda: nc.vector.tensor_tensor(out=Boh[:], in0=Boh[:], in1=bc(2), op=ALU.mult),
                   reads=[iT_b, B_b], writes=[B_b])
                for t0 in range(0, NQ, 4):
                    bk, bb = next_bank()
                    for j in range(4):
                        op("pe", lambda: nc.tensor.matmul(bk[:, j * 128:(j + 1) * 128], lhsT=Boh[:, :, t0 + j], rhs=Aoh[:, :, t0 + j],
                                                          start=True, stop=True), reads=[A_b, B_b], writes=[bb], signal=(j == 3))
                    src = bk[:].rearrange("p (j i) -> p i j", j=4)
                    dst = Gs[:, :, o + t0:o + t0 + 4]
                    op("act", lambda: nc.scalar.copy(out=dst, in_=src), reads=[bb], writes=[Gs_b])
            for c8 in range(8):
                dma("sp", gscr[c8 * 16:(c8 + 1) * 16, :, tt * 128:(tt + 1) * 128].rearrange("c p t -> p c t"),
                    Gs[:, c8 * 16:(c8 + 1) * 16, :], reads=[Gs_b], writes=[gscr_b])
        fw.barrier()
    x3 = sb("x3", [128, 8, 2048], F32, st_p)
    x3_b = [Buf() for _ in range(8)]
    for tt in range(8):
        dma("sp", x3[:, tt, :], x2s[tt * 128:(tt + 1) * 128, :], writes=[x3_b[tt]])
    NG = 8
    with ExitStack() as stm:
        AT = sb("AT", [128, NG, 1024], BF16, stm); AT_b = [Buf() for _ in range(NG)]
        vb = sb("vb", [128, NG, 2048], BF16, stm); vb_b = [Buf() for _ in range(NG)]
        gtmp = [sb(f"gtmp{i}", [128, 1024], BF16, stm) for i in range(2)]; gtmp_b = [Buf(), Buf()]
        gsl = [sb(f"gsl{i}", [128, 1024], BF16, stm) for i in range(2)]; gsl_b = [Buf() for _ in range(2)]
        NCH = 128
        SG = 4

        def U_part(c):
            cl = c % NG
            wb, wbb = wload(ut_g[c], scale=gffn)
            gi = c % 2
            dma("sp", gsl[gi][:], gscr[c, :, :], reads=[gscr_b], writes=[gsl_b[gi]])
            wload(v_g[c], dst=(vb[:, cl, :], vb_b[cl]))
            u = c % 2
            for ts in (0, 512):
                bk, bb = next_bank()
                for dc in range(16):
                    op("pe", lambda: nc.tensor.matmul(bk[:], lhsT=wb[:, dc * 128:(dc + 1) * 128], rhs=xn2T[:, dc, ts:ts + 512],
                                                      start=(dc == 0), stop=(dc == 15)), reads=[wbb] + xn2b(ts), writes=[bb], signal=(dc == 15))
                op("act", lambda: nc.scalar.activation(out=gtmp[u][:, ts:ts + 512], in_=bk[:], func=GELU), reads=[bb], writes=[gtmp_b[u]])
            op("dve", lambda: nc.vector.tensor_tensor(out=AT[:, cl, :], in0=gtmp[u][:], in1=gsl[gi][:], op=ALU.mult),
               reads=[gtmp_b[u], gsl_b[gi]], writes=[AT_b[cl]])

        def V_part(k):
            cls = [(k * SG + i) % NG for i in range(SG)]
            for tt in range(8):
                for dr in range(4):
                    bk, bb = next_bank()
                    for n_, ci_ in enumerate(cls):
                        op("pe", lambda: nc.tensor.matmul(bk[:], lhsT=AT[:, ci_, tt * 128:(tt + 1) * 128], rhs=vb[:, ci_, dr * 512:(dr + 1) * 512],
                                                          start=(n_ == 0), stop=(n_ == SG - 1)),
                           reads=[AT_b[ci_], vb_b[ci_]], writes=[bb], signal=(n_ == SG - 1))
                    dst = x3[:, tt, dr * 512:(dr + 1) * 512]
                    op("dve", lambda: nc.vector.tensor_tensor(out=dst, in0=bk[:], in1=dst, op=ALU.add), reads=[bb, x3_b[tt]], writes=[x3_b[tt]])

        nsub = NCH // SG
        for c in range(SG):
            U_part(c)
        for k in range(nsub):
            nxt = (k + 1) * SG
            if nxt < NCH:
                U_part(nxt)
            V_part(k)
            for c in range(nxt + 1, min(nxt + SG, NCH)):
                U_part(c)
        fw.barrier()
    with ExitStack() as stf:
        gf = sb("gf", [128, 2048], F32, stf); gf_b = Buf()
        dma("sp", gf[:], gfin_d[:, :], writes=[gf_b])
        jk = sb("jk", [128, 2048], BF16, stf); jk_b = Buf()
        ot = [sb(f"ot{i}", [128, 2048], F32, stf) for i in range(2)]; ot_b = [Buf(), Buf()]
        fs = sb("fs", [128, 8, 4], F32, stf); fs_b = [Buf() for _ in range(8)]
        for tt in range(8):
            u = tt % 2
            xa = x3[:, tt, :]
            op("act", lambda: nc.scalar.activation(out=jk[:], in_=xa, func=AF.Square, accum_out=fs[:, tt, 0:1]),
               reads=[x3_b[tt]], writes=[jk_b, fs_b[tt]])
            op("dve", lambda: nc.vector.tensor_scalar(out=fs[:, tt, 1:2], in0=fs[:, tt, 0:1], scalar1=1.0 / 2048, scalar2=1e-6,
                                                      op0=ALU.mult, op1=ALU.add), reads=[fs_b[tt]], writes=[fs_b[tt]])
            op("act", lambda: nc.scalar.activation(out=fs[:, tt, 2:3], in_=fs[:, tt, 1:2], func=AF.Sqrt), reads=[fs_b[tt]], writes=[fs_b[tt]])
            op("dve", lambda: nc.vector.reciprocal(out=fs[:, tt, 3:4], in_=fs[:, tt, 2:3]), reads=[fs_b[tt]], writes=[fs_b[tt]])
            op("dve", lambda: nc.vector.scalar_tensor_tensor(out=ot[u][:], in0=xa, scalar=fs[:, tt, 3:4], in1=gf[:], op0=ALU.mult, op1=ALU.mult),
               reads=[x3_b[tt], fs_b[tt], gf_b], writes=[ot_b[u]])
            dma("sp", y[tt * 128:(tt + 1) * 128, :], ot[u][:], reads=[ot_b[u]])
        fw.barrier()
    st_p.close()
    es.close()
    return nc


def rel_bucket_np(dist):
    n = np.maximum(dist, 0)
    max_exact = 16
    nf = np.maximum(n, 1).astype(np.float32)
    large = max_exact + (np.log(nf / max_exact) / math.log(128 / max_exact) * (32 - max_exact)).astype(np.int32)
    large = np.minimum(large, 31)
    return np.where(n < max_exact, n, large)


def make_inputs(inp):
    f = np.float32
    x = np.asarray(inp["x"], f)
    w_in = np.asarray(inp["w_in"], f)[0]
    cols = []
    for j in range(8):
        cols.append(4096 + j * 128)
    for j in range(8):
        cols.append(3072 + j * 128)
    for h in range(8):
        cols += [h * 128, 1024 + h * 128, 2048 + h * 128]
    for j in range(32):
        cols.append(5120 + j * 128)
    w4 = w_in.reshape(16, 128, 72, 128)
    nat = [c // 128 for c in cols]
    win_g = np.ascontiguousarray(w4[:, :, nat, :].transpose(2, 1, 0, 3)).reshape(72, 128, 2048)
    wbr = np.asarray(inp["w_branch"], f)[0]
    wbr_g = np.ascontiguousarray(wbr.reshape(2, 8, 128, 16, 128).transpose(0, 3, 2, 1, 4)).reshape(32, 128, 1024)
    wout = np.asarray(inp["w_out"], f)[0]
    wout_g = np.ascontiguousarray(wout.reshape(16, 128, 16, 128).transpose(2, 1, 0, 3)).reshape(16, 128, 2048)
    wq = np.asarray(inp["peer_wq"], f)[0]
    wq_g = np.ascontiguousarray(wq.reshape(16, 128, 16, 128).transpose(2, 1, 0, 3)).reshape(16, 128, 2048)
    U = np.asarray(inp["peer_u"], f)[0]
    ut_g = np.ascontiguousarray(U.reshape(128, 128, 16, 128).transpose(0, 3, 2, 1)).reshape(128, 128, 2048)
    v_g = np.ascontiguousarray(np.asarray(inp["peer_v"], f)[0].reshape(128, 128, 2048))
    keys = np.asarray(inp["peer_keys"], f)[0]
    keysT = np.ascontiguousarray(keys.reshape(16, 128, 128).transpose(2, 0, 1))
    cst = np.zeros((128, 512), f)
    cst[:, 0:16] = np.asarray(inp["norm_mix_g"], f)[0].reshape(16, 128).T
    cst[:, 16:32] = np.asarray(inp["norm_ffn_g"], f)[0].reshape(16, 128).T
    cw = np.asarray(inp["conv_w"], f)[0]
    cst[:, 32:64] = cw.reshape(4, 8, 128).transpose(2, 1, 0).reshape(128, 32)
    cst[:, 64:72] = np.asarray(inp["conv_b"], f)[0].reshape(8, 128).T
    cst[:, 72:80] = np.asarray(inp["lru_ba"], f)[0].reshape(8, 128).T
    cst[:, 80:88] = np.asarray(inp["lru_bx"], f)[0].reshape(8, 128).T
    cst[:, 88:96] = np.asarray(inp["lru_lambda"], f)[0].reshape(8, 128).T
    rb = np.asarray(inp["rel_bias"], f)
    cst[:, 100:108] = rb[31][None, :]
    lruw = np.stack([np.asarray(inp["lru_wa"], f)[0], np.asarray(inp["lru_wx"], f)[0]], 0)
    lruw = np.ascontiguousarray(lruw.transpose(2, 0, 1, 3))
    ident = np.eye(128, dtype=f)
    qi = np.arange(128)[:, None]
    ki = np.arange(256)[None, :]
    ownbias = np.zeros((128, 8, 2, 256), f)
    for par in range(2):
        dist = par * 128 + qi - ki
        bk = rel_bucket_np(dist)
        vals = rb[bk]
        vals = np.where((dist >= 0)[:, :, None], vals, f(NEG))
        ownbias[:, :, par, :] = vals.transpose(0, 2, 1)
    distp = 256 + qi - ki
    prevbias = np.ascontiguousarray(rb[rel_bucket_np(distp)].transpose(0, 2, 1))
    gfin = np.broadcast_to(np.asarray(inp["norm_final_g"], f)[None, :], (128, 2048)).copy()
    iota = np.broadcast_to(np.arange(128, dtype=f)[None, :], (128, 128)).copy()
    if DEBUG.get("stage", 99) < 5:
        ut_g, v_g = ut_g[:1], v_g[:1]
    shared = dict(win_g=win_g, wbr_g=wbr_g, wout_g=wout_g, wq_g=wq_g, ut_g=ut_g, v_g=v_g, keysT=keysT,
                  lruw=lruw, ident=ident, ownbias=ownbias, prevbias=prevbias, gfin=gfin, iota=iota)
    in_maps = []
    for core in range(8):
        b, half = core // 2, core % 2
        xl = np.zeros((2048, 2048), f)
        if half == 1:
            xl[:] = x[b]
        else:
            xl[1024:] = x[b, :1024]
        c2 = cst.copy()
        c2[:, 96] = float(half)
        bv = np.full((128, 8, 8), NEG, f)
        for qt in range(8):
            ob = 4 + qt // 2
            lo = 0 if half == 1 else 4
            bv[:, qt, lo:ob] = 0.0
        m = dict(shared)
        m.update(xloc=xl, cst=c2, blkvalid=bv)
        in_maps.append(m)
    return in_maps


def kernel(**inputs):
    dbg = DEBUG.get("dbg")
    nc = build_program(dbg)
    in_maps = make_inputs(inputs)
    res = run_bass_kernel_spmd(nc, in_maps, core_ids=list(range(8)))
    if dbg:
        DEBUG["res"] = res.results
    out = np.zeros((4, 2048, 2048), np.float32)
    for core in range(8):
        b, half = core // 2, core % 2
        out[b, half * 1024:(half + 1) * 1024] = res.results[core]["y"]
    return out
```

```python
import math
from contextlib import ExitStack
import numpy as np
import concourse.bass as bass
import concourse.mybir as mybir
from concourse.bass_utils import run_bass_kernel_spmd

F32 = mybir.dt.float32
BF16 = mybir.dt.bfloat16
U32 = mybir.dt.uint32
AF = mybir.ActivationFunctionType
ALU = mybir.AluOpType
AX = mybir.AxisListType

NEG = -1e30
SCALE = 128 ** -0.5
DEBUG = {}


class Buf:
    __slots__ = ("w", "r", "name")

    def __init__(self, name=""):
        self.w = None
        self.r = {}
        self.name = name


class Eng:
    def __init__(self, name, eng, sem, sid, is_pe=False):
        self.name, self.eng, self.sem, self.sid = name, eng, sem, sid
        self.n = 0
        self.waited = {}
        self.is_pe = is_pe


class FW:
    def __init__(self, nc, es):
        self.nc = nc
        self.es = es
        self.sems = {}
        self.engs = {}
        sid = 0
        for name, eng, pe in (("pe", nc.tensor, True), ("act", nc.scalar, False),
                              ("dve", nc.vector, False), ("pool", nc.gpsimd, False)):
            sem = es.enter_context(nc.semaphore("s_" + name))
            self.sems[sid] = sem
            self.engs[name] = Eng(name, eng, sem, sid, pe)
            sid += 1
        self.queues = {}
        for qname, eng, npool in (("sp", nc.sync, 24), ("gq", nc.gpsimd, 8)):
            pool = []
            for i in range(npool):
                sem = es.enter_context(nc.semaphore(f"d_{qname}{i}"))
                self.sems[sid] = sem
                pool.append([sid, 0])
                sid += 1
            self.queues[qname] = dict(eng=eng, pool=pool, k=0, waited={}, name=qname)
        self.queues["gq"]["waited"] = self.engs["pool"].waited
        self.nsem = sid

    def _wait(self, eng_obj, waited, deps):
        for sid, val in deps.items():
            if waited.get(sid, 0) < val:
                eng_obj.wait_ge(self.sems[sid], val)
                waited[sid] = val

    def _deps(self, reads, writes, self_sid, is_pe):
        deps = {}

        def add(ev):
            sid, val = ev
            if deps.get(sid, 0) < val:
                deps[sid] = val
        for b in reads:
            if b.w is not None:
                if not (is_pe and b.w[0] == self_sid):
                    add(b.w)
        for b in writes:
            if b.w is not None and not (is_pe and b.w[0] == self_sid):
                add(b.w)
            for sid, val in b.r.items():
                if not (is_pe and sid == self_sid):
                    add((sid, val))
        return deps

    def _mark(self, ev, reads, writes):
        for b in reads:
            if b.r.get(ev[0], 0) < ev[1]:
                b.r[ev[0]] = ev[1]
        for b in writes:
            b.w = ev
            b.r = {}

    def op(self, ename, fn, reads=(), writes=(), signal=True):
        e = self.engs[ename]
        deps = self._deps(reads, writes, e.sid, e.is_pe)
        if deps.get(e.sid, 0) > e.n:
            if e.n > 0:
                deps[e.sid] = e.n
            else:
                del deps[e.sid]
        self._wait(e.eng, e.waited, deps)
        ins = fn()
        if signal:
            e.n += 1
            ins.then_inc(e.sem, 1)
            ev = (e.sid, e.n)
        else:
            ev = (e.sid, e.n + 1)
        self._mark(ev, reads, writes)
        return ins

    def dma(self, qname, out, in_, reads=(), writes=()):
        q = self.queues[qname]
        slot = q["pool"][q["k"] % len(q["pool"])]
        q["k"] += 1
        deps = self._deps(reads, writes, -1, False)
        if slot[1] > 0:
            deps[slot[0]] = max(deps.get(slot[0], 0), slot[1])
        self._wait(q["eng"], q["waited"], deps)
        slot[1] += 16
        q["eng"].dma_start(out=out, in_=in_).then_inc(self.sems[slot[0]], 16)
        ev = (slot[0], slot[1])
        self._mark(ev, reads, writes)
        return ev

    def barrier(self):
        tot = {}
        for e in self.engs.values():
            if e.n:
                tot[e.sid] = e.n
        for q in self.queues.values():
            for sid, val in q["pool"]:
                if val:
                    tot[sid] = val
        for e in self.engs.values():
            self._wait(e.eng, e.waited, {s: v for s, v in tot.items() if s != e.sid or not e.is_pe})
        q = self.queues["sp"]
        self._wait(q["eng"], q["waited"], tot)


def build_program(dbg=None):
    nc = bass.Bass("TRN2", target_bir_lowering=False)
    es = ExitStack()
    fw = FW(nc, es)
    op, dma = fw.op, fw.dma

    def din(name, shape, dt=F32):
        return nc.dram_tensor(name, list(shape), dt, kind="ExternalInput").ap()

    xloc = din("xloc", [2048, 2048])
    win_g = din("win_g", [72, 128, 2048])
    wbr_g = din("wbr_g", [32, 128, 1024])
    wout_g = din("wout_g", [16, 128, 2048])
    wq_g = din("wq_g", [16, 128, 2048])
    NUV = 128 if DEBUG.get("stage", 99) >= 5 else 1
    ut_g = din("ut_g", [NUV, 128, 2048])
    v_g = din("v_g", [NUV, 128, 2048])
    keysT = din("keysT", [128, 16, 128])
    cst = din("cst", [128, 512])
    lruw = din("lruw", [128, 2, 8, 128])
    ident_d = din("ident", [128, 128])
    ownbias_d = din("ownbias", [128, 8, 2, 256])
    prevbias_d = din("prevbias", [128, 8, 256])
    blkvalid_d = din("blkvalid", [128, 8, 8])
    gfin_d = din("gfin", [128, 2048])
    iota_d = din("iota", [128, 128])
    y = nc.dram_tensor("y", [1024, 2048], F32, kind="ExternalOutput").ap()
    gscr = nc.dram_tensor("gscr", [128, 128, 1024], BF16, kind="Internal").ap()
    x2s = nc.dram_tensor("x2s", [1024, 2048], F32, kind="Internal").ap()
    x2s_b = Buf()
    gscr_b = Buf()
    dbg_out = {}
    if dbg:
        for k, shp in dbg.items():
            dbg_out[k] = nc.dram_tensor("dbg_" + k, list(shp), F32, kind="ExternalOutput").ap()

    def sb(name, shape, dt=F32, stack=es):
        return stack.enter_context(nc.sbuf_tensor(name, list(shape), dt))

    banks = [es.enter_context(nc.psum_tensor(f"ps{i}", [128, 512], F32)) for i in range(8)]
    bank_bufs = [Buf(f"ps{i}") for i in range(8)]
    bank_k = [0]

    def next_bank():
        i = bank_k[0] % 8
        bank_k[0] += 1
        return banks[i], bank_bufs[i]

    cst_sb = sb("cst_sb", [128, 512]); cst_b = Buf()
    ident_f = sb("ident_f", [128, 128]); ident_b = Buf()
    ident_h = sb("ident_h", [128, 128], BF16)
    dma("sp", cst_sb[:], cst[:, :], writes=[cst_b])
    dma("sp", ident_f[:], ident_d[:, :], writes=[ident_b])
    op("dve", lambda: nc.vector.tensor_copy(out=ident_h[:], in_=ident_f[:]), reads=[ident_b], writes=[ident_b])
    C_GMIX, C_GFFN, C_CONVW, C_CONVB, C_BA, C_BX, C_LAM, C_FLAG, C_B31 = 0, 16, 32, 64, 72, 80, 88, 96, 100
    gmix = cst_sb[:, C_GMIX:C_GMIX + 16]
    gffn = cst_sb[:, C_GFFN:C_GFFN + 16]

    NST = 3
    NSF = 2
    wst = [sb(f"wst{i}", [128, 2048]) for i in range(NSF)]
    wst_b = [Buf() for _ in range(NSF)]
    wbf = [sb(f"wbf{i}", [128, 2048], BF16) for i in range(NST)]
    wbf_b = [Buf() for _ in range(NST)]
    wk = [0]

    def wload(src_ap, ncols=2048, scale=None, dst=None):
        i = wk[0] % NST
        f = wk[0] % NSF
        wk[0] += 1
        dma("sp", wst[f][:, 0:ncols], src_ap, writes=[wst_b[f]])
        ename = "pool" if (wk[0] % 2) else "dve"
        eng = nc.gpsimd if ename == "pool" else nc.vector
        if scale is not None:
            nk = ncols // 128
            o = wbf[i][:, 0:ncols].rearrange("p (c k) -> p c k", k=128)
            a = wst[f][:, 0:ncols].rearrange("p (c k) -> p c k", k=128)
            s = scale.unsqueeze(2).to_broadcast([128, nk, 128])
            op(ename, lambda: eng.tensor_tensor(out=o, in0=a, in1=s, op=ALU.mult),
               reads=[wst_b[f], cst_b], writes=[wbf_b[i]])
        elif dst is not None:
            op(ename, lambda: eng.tensor_copy(out=dst[0], in_=wst[f][:, 0:ncols]), reads=[wst_b[f]], writes=[dst[1]])
            return dst
        else:
            op(ename, lambda: eng.tensor_copy(out=wbf[i][:, 0:ncols], in_=wst[f][:, 0:ncols]),
               reads=[wst_b[f]], writes=[wbf_b[i]])
        return wbf[i], wbf_b[i]

    def dbg_dump(key, ap_sb, buf, dram_ap=None):
        if key in dbg_out:
            dma("sp", dbg_out[key][:] if dram_ap is None else dram_ap, ap_sb, reads=[buf])

    st_br = ExitStack()
    orecT = sb("orecT", [128, 8, 1024], BF16, st_br)
    orec_b = [Buf() for _ in range(8)]
    oattT = sb("oattT", [128, 8, 1024], BF16, st_br)
    oattT_b = Buf()
    mg_b = [Buf() for _ in range(16)]
    st_xo = ExitStack()
    st_xp = ExitStack()
    xnT_o = sb("xnT_o", [128, 16, 1024], BF16, st_xo)
    xnT_p = sb("xnT_p", [128, 16, 1024], BF16, st_xp)

    def xnT(c, t0, n):
        return xnT_p[:, c, t0:t0 + n] if t0 < 1024 else xnT_o[:, c, t0 - 1024:t0 - 1024 + n]

    def xnT8(c0, t0, n):
        return xnT_p[:, c0:c0 + 8, t0:t0 + n] if t0 < 1024 else xnT_o[:, c0:c0 + 8, t0 - 1024:t0 - 1024 + n]
    xnT_b = [Buf(f"xnT{t}") for t in range(16)]

    def norm_transpose(src_rows, dstT, dst_bufs, ntiles, stack, tag, src_is_sbuf=None):
        xin = [sb(f"{tag}xin{i}", [128, 2048], F32, stack) for i in range(2)]
        xin_b = [Buf(), Buf()]
        junk = sb(f"{tag}junk", [128, 2048], BF16, stack); junk_b = Buf()
        xs = [sb(f"{tag}xs{i}", [128, 2048], BF16, stack) for i in range(2)]
        xs_b = [Buf(), Buf()]
        st = sb(f"{tag}st", [128, 2, 4], F32, stack)
        st_b = [Buf(), Buf()]
        for tt in range(ntiles):
            s = tt % 2
            if src_is_sbuf is None:
                dma("sp", xin[s][:], src_rows(tt), writes=[xin_b[s]])
                xa, xb_ = xin[s][:], xin_b[s]
            else:
                xa, xb_ = src_is_sbuf(tt)
            op("act", lambda: nc.scalar.activation(out=junk[:], in_=xa, func=AF.Square, accum_out=st[:, s, 0:1]),
               reads=[xb_], writes=[junk_b, st_b[s]])
            op("dve", lambda: nc.vector.tensor_scalar(
                out=st[:, s, 1:2], in0=st[:, s, 0:1], scalar1=1.0 / 2048, scalar2=1e-6,
                op0=ALU.mult, op1=ALU.add), reads=[st_b[s]], writes=[st_b[s]])
            op("act", lambda: nc.scalar.activation(out=st[:, s, 2:3], in_=st[:, s, 1:2], func=AF.Sqrt),
               reads=[st_b[s]], writes=[st_b[s]])
            op("dve", lambda: nc.vector.reciprocal(out=st[:, s, 3:4], in_=st[:, s, 2:3]),
               reads=[st_b[s]], writes=[st_b[s]])
            op("dve", lambda: nc.vector.tensor_scalar(
                out=xs[s][:], in0=xa, scalar1=st[:, s, 3:4], scalar2=None, op0=ALU.mult),
               reads=[xb_, st_b[s]], writes=[xs_b[s]])
            for half in range(2):
                bk, bb = next_bank()
                bkh = bk[:].bitcast(BF16)
                for j in range(8):
                    c = half * 8 + j
                    op("pe", lambda: nc.tensor.transpose(
                        out=bkh[:, j * 128:(j + 1) * 128], in_=xs[s][:, c * 128:(c + 1) * 128],
                        identity=ident_h[:]), reads=[xs_b[s], ident_b], writes=[bb], signal=(j == 7))
                src = bkh.rearrange("p (c k) -> p c k", k=128)
                dst = dstT(half * 8, tt * 128, 128)
                if half == 0:
                    op("act", lambda: nc.scalar.copy(out=dst, in_=src), reads=[bb], writes=[dst_bufs[tt]])
                else:
                    op("dve", lambda: nc.vector.tensor_copy(out=dst, in_=src), reads=[bb], writes=[dst_bufs[tt]])

    with ExitStack() as st1:
        norm_transpose(lambda tt: xloc[tt * 128:(tt + 1) * 128, :], xnT8, xnT_b, 16, st1, "n1")
        fw.barrier()

    xnb = lambda ts: [xnT_b[t] for t in range(ts // 128, ts // 128 + 4)]
    def proj_fm(wb, wbb, srcT, src_bufs, nk, t0, t1, evac):
        for ts in range(t0, t1, 512):
            bk, bb = next_bank()
            rb = src_bufs(ts) if callable(src_bufs) else src_bufs
            for c in range(nk):
                op("pe", lambda: nc.tensor.matmul(bk[:], lhsT=wb[:, c * 128:(c + 1) * 128],
                                                  rhs=(srcT(c, ts, 512) if callable(srcT) else srcT[:, c, ts:ts + 512]), start=(c == 0), stop=(c == nk - 1)),
                   reads=[wbb] + rb, writes=[bb], signal=(c == nk - 1))
            evac(bk, bb, ts)

    GELU = AF.Gelu_apprx_tanh
    yrh = {0: wload(win_g[0], scale=gmix), 1: wload(win_g[1], scale=gmix)}
    for cc in range(8):
        if cc + 2 < 8:
            yrh[cc + 2] = wload(win_g[cc + 2], scale=gmix)
        wb, wbb = yrh.pop(cc)

        def ev_yr(bk, bb, ts, cc=cc):
            op("act", lambda: nc.scalar.activation(out=orecT[:, cc, ts - 1024:ts - 1024 + 512], in_=bk[:], func=GELU),
               reads=[bb], writes=[orec_b[cc]])
        proj_fm(wb, wbb, xnT, xnb, 16, 1024, 2048, ev_yr)

    with ExitStack() as st2:
        lw_f = sb("lw_f", [128, 2, 8, 128], F32, st2); lw_b = Buf()
        lw_h = sb("lw_h", [128, 2, 8, 128], BF16, st2)
        dma("sp", lw_f[:], lruw[:, :, :, :], writes=[lw_b])
        op("dve", lambda: nc.vector.tensor_copy(out=lw_h[:], in_=lw_f[:]), reads=[lw_b], writes=[lw_b])
        csp = sb("csp", [128, 4, 8], F32, st2); csp_b = Buf()
        op("act", lambda: nc.scalar.activation(out=csp[:, 0, :], in_=cst_sb[:, C_LAM:C_LAM + 8], func=AF.Exp, scale=-1.0),
           reads=[cst_b], writes=[csp_b])
        op("act", lambda: nc.scalar.activation(out=csp[:, 1, :], in_=csp[:, 0, :], func=AF.Ln, bias=1.0),
           reads=[csp_b], writes=[csp_b])
        op("dve", lambda: nc.vector.tensor_scalar(out=csp[:, 2, :], in0=csp[:, 1, :], scalar1=-8.0, scalar2=None, op0=ALU.mult),
           reads=[csp_b], writes=[csp_b])
        op("dve", lambda: nc.vector.tensor_scalar(out=csp[:, 3, :], in0=csp[:, 1, :], scalar1=-16.0, scalar2=None, op0=ALU.mult),
           reads=[csp_b], writes=[csp_b])
        T = 2048
        H = 1024
        xr = [sb(f"xr{i}", [128, T + 3], F32, st2) for i in range(2)]; xr_b = [Buf(), Buf()]
        for i in range(2):
            op("dve", lambda: nc.vector.memset(xr[i][:, 0:3], 0.0), writes=[xr_b[i]])
        tset = []
        for hf in range(2):
            d = {}
            for nm, dt_ in (("xc", F32), ("xch", BF16), ("rg", F32), ("ig", F32), ("sq", F32), ("hh", F32)):
                d[nm] = sb(f"{nm}{hf}", [128, H], dt_, st2)
                d[nm + "_b"] = Buf()
            tset.append(d)
        flag = cst_sb[:, C_FLAG:C_FLAG + 1]
        def xr_proj_gen(cc):
            wb, wbb = wload(win_g[8 + cc], scale=gmix)
            X, Xb = xr[cc % 2], xr_b[cc % 2]
            yield
            for ts in range(0, 2048, 512):
                bk, bb = next_bank()
                for c in range(16):
                    op("pe", lambda: nc.tensor.matmul(bk[:], lhsT=wb[:, c * 128:(c + 1) * 128], rhs=xnT(c, ts, 512),
                                                      start=(c == 0), stop=(c == 15)), reads=[wbb] + xnb(ts), writes=[bb], signal=(c == 15))
                if (ts // 512) % 2 == 0:
                    op("act", lambda: nc.scalar.copy(out=X[:, 3 + ts:3 + ts + 512], in_=bk[:]), reads=[bb], writes=[Xb])
                else:
                    op("dve", lambda: nc.vector.tensor_copy(out=X[:, 3 + ts:3 + ts + 512], in_=bk[:]), reads=[bb], writes=[Xb])
                yield

        def lru_chain(cc, hf):
            X, Xb = xr[cc % 2], xr_b[cc % 2]
            cw = lambda j: cst_sb[:, C_CONVW + cc * 4 + j:C_CONVW + cc * 4 + j + 1]
            cb = cst_sb[:, C_CONVB + cc:C_CONVB + cc + 1]
            d = tset[hf]
            o = hf * H
            xc, xc_b, xch, xch_b = d["xc"], d["xc_b"], d["xch"], d["xch_b"]
            rg, rg_b, ig, ig_b, sq, sq_b, hh, hh_b = d["rg"], d["rg_b"], d["ig"], d["ig_b"], d["sq"], d["sq_b"], d["hh"], d["hh_b"]
            aa, aa_b = rg, rg_b
            op("dve", lambda: nc.vector.tensor_scalar(out=xc[:], in0=X[:, o:o + H], scalar1=cw(0), scalar2=cb,
                                                      op0=ALU.mult, op1=ALU.add), reads=[Xb, cst_b], writes=[xc_b]); yield
            for j in range(1, 4):
                op("dve", lambda: nc.vector.scalar_tensor_tensor(out=xc[:], in0=X[:, o + j:o + j + H], scalar=cw(j), in1=xc[:],
                                                                 op0=ALU.mult, op1=ALU.add), reads=[Xb, xc_b, cst_b], writes=[xc_b]); yield
            op("act", lambda: nc.scalar.copy(out=xch[:], in_=xc[:]), reads=[xc_b], writes=[xch_b]); yield
            for gi, (dst, dstb, bcol) in enumerate(((rg, rg_b, C_BA), (ig, ig_b, C_BX))):
                for ts in range(0, H, 512):
                    bk, bb = next_bank()
                    op("pe", lambda: nc.tensor.matmul(bk[:], lhsT=lw_h[:, gi, cc, :], rhs=xch[:, ts:ts + 512], start=True, stop=True),
                       reads=[lw_b, xch_b], writes=[bb])
                    op("act", lambda: nc.scalar.activation(out=dst[:, ts:ts + 512], in_=bk[:], func=AF.Sigmoid,
                                                           bias=cst_sb[:, bcol + cc:bcol + cc + 1]), reads=[bb, cst_b], writes=[dstb])
                    yield
            op("act", lambda: nc.scalar.activation(out=sq[:], in_=rg[:], func=AF.Exp, scale=csp[:, 3, cc:cc + 1]),
               reads=[rg_b, csp_b], writes=[sq_b]); yield
            op("act", lambda: nc.scalar.activation(out=aa[:], in_=rg[:], func=AF.Exp, scale=csp[:, 2, cc:cc + 1]),
               reads=[rg_b, csp_b], writes=[aa_b]); yield
            op("act", lambda: nc.scalar.activation(out=sq[:], in_=sq[:], func=AF.Sqrt, scale=-1.0, bias=1.0),
               reads=[sq_b], writes=[sq_b]); yield
            if hf == 0:
                op("dve", lambda: nc.vector.scalar_tensor_tensor(out=ig[:], in0=ig[:], scalar=flag, in1=xc[:],
                                                                 op0=ALU.mult, op1=ALU.mult), reads=[ig_b, xc_b, cst_b], writes=[ig_b])
            else:
                op("dve", lambda: nc.vector.tensor_tensor(out=ig[:], in0=ig[:], in1=xc[:], op=ALU.mult),
                   reads=[ig_b, xc_b], writes=[ig_b])
            yield
            op("dve", lambda: nc.vector.tensor_tensor(out=sq[:], in0=sq[:], in1=ig[:], op=ALU.mult),
               reads=[sq_b, ig_b], writes=[sq_b]); yield
            if hf == 0:
                op("dve", lambda: nc.vector.tensor_tensor_scan(out=hh[:], data0=aa[:], data1=sq[:], initial=0.0,
                                                               op0=ALU.mult, op1=ALU.add), reads=[aa_b, sq_b], writes=[hh_b])
                yield
            else:
                h0, h0_b = tset[0]["hh"], tset[0]["hh_b"]
                op("dve", lambda: nc.vector.tensor_tensor_scan(out=hh[:], data0=aa[:], data1=sq[:], initial=h0[:, H - 1:H],
                                                               op0=ALU.mult, op1=ALU.add), reads=[aa_b, sq_b, h0_b], writes=[hh_b])
                yield
                op("dve", lambda: nc.vector.tensor_tensor(out=orecT[:, cc, :], in0=orecT[:, cc, :], in1=hh[:], op=ALU.mult),
                   reads=[hh_b, orec_b[cc]], writes=[orec_b[cc]])
                yield

        tasks = []
        for cc in range(8):
            dp = []
            if cc >= 1:
                dp.append(("P", cc - 1))
            if cc >= 2:
                dp += [("C", cc - 2, 0), ("C", cc - 2, 1)]
            tasks.append((("P", cc), (lambda cc=cc: xr_proj_gen(cc)), dp))
            d0 = [("P", cc)] + ([("C", cc - 1, 0)] if cc >= 1 else [])
            d1 = d0 + ([("C", cc - 1, 1)] if cc >= 1 else [])
            tasks.append((("C", cc, 0), (lambda cc=cc: lru_chain(cc, 0)), d0))
            tasks.append((("C", cc, 1), (lambda cc=cc: lru_chain(cc, 1)), d1))
        done_t = set()
        active = []
        pending = list(tasks)
        while pending or active:
            for t in list(pending):
                if all(dd in done_t for dd in t[2]):
                    active.append((t[0], t[1]()))
                    pending.remove(t)
            assert active, "scheduler stuck"
            for item in list(active):
                try:
                    next(item[1])
                except StopIteration:
                    active.remove(item)
                    done_t.add(item[0])
        fw.barrier()

    if "orecT" in dbg_out:
        with ExitStack() as sd:
            tmp = sb("dbgtmp", [128, 8, 1024], F32, sd); tb = Buf()
            op("dve", lambda: nc.vector.tensor_copy(out=tmp[:], in_=orecT[:]), reads=orec_b, writes=[tb])
            dma("sp", dbg_out["orecT"][:, :, :], tmp[:], reads=[tb])
            fw.barrier()

    STAGE = DEBUG.get("stage", 99)

    def finish_zero():
        with ExitStack() as sd:
            z = sb("zz", [128, 2048], F32, sd); zb = Buf()
            op("dve", lambda: nc.vector.memset(z[:], 0.0), writes=[zb])
            for t in range(8):
                dma("sp", y[t * 128:(t + 1) * 128, :], z[:], reads=[zb])
            fw.barrier()
        st_xp.close(); st_xo.close(); st_br.close()
        es.close()
        return nc

    if STAGE <= 2:
        return finish_zero()

    b31 = cst_sb[:, C_B31:C_B31 + 8]
    with ExitStack() as st3:
        oatt2 = [sb(f"oatt{i}", [128, 8, 128], BF16, st3) for i in range(2)]; oatt_b2 = [Buf(), Buf()]
        ownb2 = [sb(f"ownb{i}", [128, 2, 256], F32, st3) for i in range(2)]; ownb_b2 = [Buf(), Buf()]
        prevb2 = [sb(f"prevb{i}", [128, 256], F32, st3) for i in range(2)]; prevb_b2 = [Buf(), Buf()]
        blkv = sb("blkv", [128, 8, 8], F32, st3); blkv_b = Buf()
        dma("sp", blkv[:], blkvalid_d[:, :, :], writes=[blkv_b])
        qT = [sb(f"qT{i}", [128, 1024], BF16, st3) for i in range(2)]; qT_b = [Buf(), Buf()]
        kT = [sb(f"kT{i}", [128, 2056], BF16, st3) for i in range(2)]; kT_b = [Buf(), Buf()]
        vE = [sb(f"vE{i}", [128, 16, 130], BF16, st3) for i in range(2)]; vE_b = [Buf(), Buf()]
        km = sb("km", [128, 2, 8], F32, st3); km_b = [Buf(), Buf()]
        NL = 3
        S_sb = [sb(f"S{i}", [128, 2048], F32, st3) for i in range(NL)]; S_b = [Buf() for _ in range(NL)]
        P_sb = [sb(f"P{i}", [128, 2048], BF16, st3) for i in range(NL)]; P_b = [Buf() for _ in range(NL)]
        PT = [sb(f"PT{i}", [128, 16, 128], BF16, st3) for i in range(NL)]; PT_b = [Buf() for _ in range(NL)]
        sm = [sb(f"sm{i}", [128, 64], F32, st3) for i in range(NL)]; sm_b = [Buf() for _ in range(NL)]
        for i in range(2):
            op("dve", lambda: nc.vector.memset(vE[i][:, :, 128:130], 1.0), writes=[vE_b[i]])
        def run_rr(gens):
            alive = [True] * len(gens)
            while any(alive):
                for gi_ in range(len(gens)):
                    if alive[gi_]:
                        try:
                            next(gens[gi_])
                        except StopIteration:
                            alive[gi_] = False

        free_banks = list(range(8))

        def acquire(n):
            while len(free_banks) < n:
                yield None
            got = [free_banks.pop(0) for _ in range(n)]
            return [(banks[i], bank_bufs[i], i) for i in got]

        def release(lst):
            for (_, _, i) in lst:
                free_banks.append(i)

        def proj_head(h):
            s = h % 2
            ownb, ownb_b, prevb, prevb_b = ownb2[s], ownb_b2[s], prevb2[s], prevb_b2[s]
            dma("sp", ownb[:], ownbias_d[:, h, :, :], writes=[ownb_b])
            dma("sp", prevb[:], prevbias_d[:, h, :], writes=[prevb_b])
            wb, wbb = wload(win_g[16 + 3 * h], scale=gmix)
            yield
            for ts in (1024, 1536):
                L = yield from acquire(1)
                bk, bb, _ = L[0]
                for c in range(16):
                    op("pe", lambda: nc.tensor.matmul(bk[:], lhsT=wb[:, c * 128:(c + 1) * 128], rhs=xnT(c, ts, 512),
                                                      start=(c == 0), stop=(c == 15)), reads=[wbb] + xnb(ts), writes=[bb], signal=(c == 15))
                    if c % 4 == 3:
                        yield
                op("act", lambda: nc.scalar.copy(out=qT[s][:, ts - 1024:ts - 512], in_=bk[:]), reads=[bb], writes=[qT_b[s]])
                release(L)
                yield
            wb, wbb = wload(win_g[16 + 3 * h + 1], scale=gmix)
            yield
            for ts in range(0, 2048, 512):
                L = yield from acquire(1)
                bk, bb, _ = L[0]
                for c in range(16):
                    op("pe", lambda: nc.tensor.matmul(bk[:], lhsT=wb[:, c * 128:(c + 1) * 128], rhs=xnT(c, ts, 512),
                                                      start=(c == 0), stop=(c == 15)), reads=[wbb] + xnb(ts), writes=[bb], signal=(c == 15))
                    if c % 4 == 3:
                        yield
                op("act", lambda: nc.scalar.copy(out=kT[s][:, ts:ts + 512], in_=bk[:]), reads=[bb], writes=[kT_b[s]])
                release(L)
                yield
            op("dve", lambda: nc.vector.tensor_reduce(out=km[:, s, :], in_=kT[s][:, 0:2048].rearrange("p (b k) -> p b k", k=256),
                                                      axis=AX.X, op=ALU.add), reads=[kT_b[s]], writes=[km_b[s]])
            yield
            op("dve", lambda: nc.vector.tensor_scalar(out=kT[s][:, 2048:2056], in0=km[:, s, :], scalar1=1.0 / 256, scalar2=None,
                                                      op0=ALU.mult), reads=[km_b[s]], writes=[kT_b[s]])
            wb, wbb = wload(win_g[16 + 3 * h + 2], scale=gmix)
            yield
            for tg in range(4):
                L = yield from acquire(1)
                bk, bb, _ = L[0]
                for j in range(4):
                    tile = tg * 4 + j
                    for c in range(16):
                        op("pe", lambda: nc.tensor.matmul(bk[:, j * 128:(j + 1) * 128], lhsT=xnT(c, tile * 128, 128),
                                                          rhs=wb[:, c * 128:(c + 1) * 128], start=(c == 0), stop=(c == 15)),
                           reads=[wbb, xnT_b[tile]], writes=[bb], signal=(c == 15 and j == 3))
                        if c % 4 == 3:
                            yield
                src = bk[:].rearrange("p (a k) -> p a k", k=128)
                dst = vE[s][:, tg * 4:(tg + 1) * 4, 0:128]
                op("act", lambda: nc.scalar.copy(out=dst, in_=src), reads=[bb], writes=[vE_b[s]])
                release(L)
                yield

        def attn_lane(h, u):
            s = h % 2
            ownb, ownb_b, prevb, prevb_b = ownb2[s], ownb_b2[s], prevb2[s], prevb_b2[s]
            for qt in range(u, 8, NL):
                ob = 4 + qt // 2
                par = qt % 2
                nk = 256 * (ob + 1)
                lq = qT[s][:, qt * 128:(qt + 1) * 128]
                nsb = (nk + 511) // 512
                L1 = yield from acquire(nsb + 1)
                bg, bgb, _ = L1[0]
                op("pe", lambda: nc.tensor.matmul(bg[:, 0:8], lhsT=lq, rhs=kT[s][:, 2048:2056], start=True, stop=True),
                   reads=[qT_b[s], kT_b[s]], writes=[bgb])
                sbanks = []
                for kr in range(0, nk, 512):
                    w = min(512, nk - kr)
                    bk, bb, _ = L1[1 + kr // 512]
                    op("pe", lambda: nc.tensor.matmul(bk[:, 0:w], lhsT=lq, rhs=kT[s][:, kr:kr + w], start=True, stop=True),
                       reads=[qT_b[s], kT_b[s]], writes=[bb])
                    sbanks.append((bk, bb))
                yield
                m, mb = sm[u], sm_b[u]
                gm, top8, thr, selm, madd, madd2 = m[:, 0:8], m[:, 8:16], m[:, 16:17], m[:, 24:32], m[:, 32:40], m[:, 40:48]
                op("dve", lambda: nc.vector.tensor_tensor(out=gm, in0=bg[:, 0:8], in1=blkv[:, qt, :], op=ALU.add),
                   reads=[bgb, blkv_b], writes=[mb]); yield
                op("dve", lambda: nc.vector.max(out=top8, in_=gm), reads=[mb], writes=[mb]); yield
                op("dve", lambda: nc.vector.tensor_scalar(out=thr, in0=top8[:, 2:3], scalar1=-1e29, scalar2=None, op0=ALU.max),
                   reads=[mb], writes=[mb]); yield
                op("dve", lambda: nc.vector.tensor_scalar(out=selm, in0=gm, scalar1=thr, scalar2=None, op0=ALU.is_ge),
                   reads=[mb], writes=[mb]); yield
                op("dve", lambda: nc.vector.tensor_scalar(out=madd, in0=selm, scalar1=-1.0, scalar2=1e30, op0=ALU.add, op1=ALU.mult),
                   reads=[mb], writes=[mb]); yield
                op("dve", lambda: nc.vector.tensor_scalar(out=madd2, in0=madd, scalar1=b31[:, h:h + 1], scalar2=None, op0=ALU.add),
                   reads=[mb, cst_b], writes=[mb]); yield
                S, Sb = S_sb[u], S_b[u]
                for blk in range(ob + 1):
                    bk, bb = sbanks[blk // 2]
                    src = bk[:, (blk % 2) * 256:(blk % 2) * 256 + 256]
                    dstS = S[:, blk * 256:(blk + 1) * 256]
                    if blk < ob:
                        near = (blk == ob - 1 and par == 0)
                        ma = madd if near else madd2
                        if near:
                            op("dve", lambda: nc.vector.tensor_scalar(out=dstS, in0=src, scalar1=SCALE, scalar2=ma[:, blk:blk + 1],
                                                                      op0=ALU.mult, op1=ALU.add), reads=[bb, mb], writes=[Sb])
                        else:
                            op("act", lambda: nc.scalar.activation(out=dstS, in_=src, func=AF.Identity, scale=SCALE, bias=ma[:, blk:blk + 1]),
                               reads=[bb, mb], writes=[Sb])
                        if near:
                            op("dve", lambda: nc.vector.tensor_tensor(out=dstS, in0=dstS, in1=prevb[:], op=ALU.add),
                               reads=[Sb, prevb_b], writes=[Sb])
                    else:
                        op("dve", lambda: nc.vector.scalar_tensor_tensor(out=dstS, in0=src, scalar=SCALE, in1=ownb[:, par, :],
                                                                         op0=ALU.mult, op1=ALU.add), reads=[bb, ownb_b], writes=[Sb])
                    yield
                release(L1)
                op("dve", lambda: nc.vector.tensor_reduce(out=m[:, 48:49], in_=S[:, 0:nk], axis=AX.X, op=ALU.max),
                   reads=[Sb], writes=[mb]); yield
                op("dve", lambda: nc.vector.tensor_scalar(out=m[:, 49:50], in0=m[:, 48:49], scalar1=-1.0, scalar2=None, op0=ALU.mult),
                   reads=[mb], writes=[mb]); yield
                op("act", lambda: nc.scalar.activation(out=P_sb[u][:, 0:nk], in_=S[:, 0:nk], func=AF.Exp, bias=m[:, 49:50], scale=1.0),
                   reads=[Sb, mb], writes=[P_b[u]]); yield
                nkt = nk // 128
                for k0 in range(0, nkt, 8):
                    L2 = yield from acquire(1)
                    bk, bb, _ = L2[0]
                    bkh = bk[:].bitcast(BF16)
                    n8 = min(8, nkt - k0)
                    for j in range(n8):
                        kt = k0 + j
                        op("pe", lambda: nc.tensor.transpose(out=bkh[:, j * 128:(j + 1) * 128], in_=P_sb[u][:, kt * 128:(kt + 1) * 128],
                                                             identity=ident_h[:]), reads=[P_b[u], ident_b], writes=[bb], signal=(j == n8 - 1))
                    src = bkh[:, 0:n8 * 128].rearrange("p (c k) -> p c k", k=128)
                    dst = PT[u][:, k0:k0 + n8, :]
                    op("act", lambda: nc.scalar.copy(out=dst, in_=src), reads=[bb], writes=[PT_b[u]])
                    release(L2)
                    yield
                L3 = yield from acquire(1)
                bo, bob, _ = L3[0]
                for kt in range(nkt):
                    op("pe", lambda: nc.tensor.matmul(bo[:, 0:129], lhsT=PT[u][:, kt, :], rhs=vE[s][:, kt, 0:129],
                                                      start=(kt == 0), stop=(kt == nkt - 1)),
                       reads=[PT_b[u], vE_b[s]], writes=[bob], signal=(kt == nkt - 1))
                yield
                op("dve", lambda: nc.vector.reciprocal(out=m[:, 50:51], in_=bo[:, 128:129]), reads=[bob], writes=[mb]); yield
                op("dve", lambda: nc.vector.tensor_scalar(out=oatt2[s][:, qt, :], in0=bo[:, 0:128], scalar1=m[:, 50:51],
                                                          scalar2=None, op0=ALU.mult), reads=[bob, mb], writes=[oatt_b2[s]])
                release(L3)
                yield

        def oatt_T(h):
            s_ = h % 2
            L = yield from acquire(1)
            bk, bb, _ = L[0]
            bkh = bk[:].bitcast(BF16)
            for qt in range(8):
                op("pe", lambda: nc.tensor.transpose(out=bkh[:, qt * 128:(qt + 1) * 128], in_=oatt2[s_][:, qt, :],
                                                     identity=ident_h[:]), reads=[oatt_b2[s_], ident_b], writes=[bb], signal=(qt == 7))
            op("act", lambda: nc.scalar.copy(out=oattT[:, h, :], in_=bkh), reads=[bb], writes=[oattT_b])
            release(L)
            yield

        run_rr([proj_head(0)])
        for h in range(8):
            gens = [attn_lane(h, u) for u in range(NL)]
            if h < 7:
                gens.append(proj_head(h + 1))
            if h >= 1:
                gens.append(oatt_T(h - 1))
            run_rr(gens)
        run_rr([oatt_T(7)])
        fw.barrier()

    if "oattT" in dbg_out:
        with ExitStack() as sd:
            tmp = sb("dbgtmp2", [128, 8, 1024], F32, sd); tb = Buf()
            op("dve", lambda: nc.vector.tensor_copy(out=tmp[:], in_=oattT[:]), reads=[oattT_b], writes=[tb])
            dma("sp", dbg_out["oattT"][:, :, :], tmp[:], reads=[tb])
            fw.barrier()
    if STAGE <= 3:
        return finish_zero()

    st_xp.close()
    st_mg = ExitStack()
    mergedT = sb("mergedT", [128, 16, 1024], BF16, st_mg)
    with ExitStack() as st4:
        sg = [sb(f"sg{i}", [128, 1024], F32, st4) for i in range(2)]; sg_b = [Buf(), Buf()]
        tm = sb("tm", [128, 1024], F32, st4); tm_b = Buf()
        sgB = [sb(f"sgB{i}", [128, 1024], F32, st4) for i in range(2)]; sgB_b = [Buf(), Buf()]
        tmB = sb("tmB", [128, 1024], F32, st4); tmB_b = Buf()
        SG2 = [(sg, sg_b), (sgB, sgB_b)]
        TM2 = [(tm, tm_b), (tmB, tmB_b)]
        items = []
        for j in range(16):
            for n in range(2):
                items.append(("g", j, n))
            for n in range(2):
                items.append(("b", j, n))

        def load_item(it_):
            kind, j, n = it_
            if kind == "g":
                return wload(win_g[40 + n * 16 + j], scale=gmix)
            return wload(wbr_g[n * 16 + j], ncols=1024)

        handles = {}
        for i0 in range(min(2, len(items))):
            handles[i0] = load_item(items[i0])
        for i_, it_ in enumerate(items):
            if i_ + 2 < len(items):
                handles[i_ + 2] = load_item(items[i_ + 2])
            wb, wbb = handles.pop(i_)
            kind, j, n = it_
            sgs, sgs_b = SG2[j % 2]
            tmj, tmj_b = TM2[j % 2]
            if kind == "g":
                def ev_g(bk, bb, ts, n=n, sgs=sgs, sgs_b=sgs_b):
                    op("act", lambda: nc.scalar.activation(out=sgs[n][:, ts - 1024:ts - 512], in_=bk[:], func=AF.Sigmoid),
                       reads=[bb], writes=[sgs_b[n]])
                proj_fm(wb, wbb, xnT, xnb, 16, 1024, 2048, ev_g)
            else:
                srcT, srcb = (oattT, [oattT_b]) if n == 0 else (orecT, orec_b)

                def ev_b(bk, bb, ts, n=n, j=j, sgs=sgs, sgs_b=sgs_b, tmj=tmj, tmj_b=tmj_b):
                    if n == 0:
                        op("dve", lambda: nc.vector.tensor_tensor(out=tmj[:, ts:ts + 512], in0=bk[:], in1=sgs[0][:, ts:ts + 512], op=ALU.mult),
                           reads=[bb, sgs_b[0]], writes=[tmj_b])
                    else:
                        op("dve", lambda: nc.vector.tensor_tensor(out=sgs[1][:, ts:ts + 512], in0=bk[:], in1=sgs[1][:, ts:ts + 512], op=ALU.mult),
                           reads=[bb, sgs_b[1]], writes=[sgs_b[1]])
                        op("dve", lambda: nc.vector.tensor_tensor(out=mergedT[:, j, ts:ts + 512], in0=tmj[:, ts:ts + 512],
                                                                  in1=sgs[1][:, ts:ts + 512], op=ALU.add),
                           reads=[tmj_b, sgs_b[1]], writes=[mg_b[j]])
                proj_fm(wb, wbb, srcT, srcb, 8, 0, 1024, ev_b)
        fw.barrier()
    with ExitStack() as st5:
        xsl = [sb(f"xsl{i}", [128, 4, 128], F32, st5) for i in range(4)]; xsl_b = [Buf() for _ in range(4)]
        k5 = 0
        woh = {0: wload(wout_g[0]), 1: wload(wout_g[1])}
        for g in range(16):
            if g + 2 < 16:
                woh[g + 2] = wload(wout_g[g + 2])
            wb, wbb = woh.pop(g)
            for hb in range(2):
                u = k5 % 4
                k5 += 1
                dview = lambda t: t[1024 * 0 + hb * 512:hb * 512 + 512, g * 128:(g + 1) * 128].rearrange("(k p) c -> p k c", p=128)
                dma("sp", xsl[u][:], xloc[1024 + hb * 512:1024 + hb * 512 + 512, g * 128:(g + 1) * 128].rearrange("(k p) c -> p k c", p=128),
                    writes=[xsl_b[u]])
                bk, bb = next_bank()
                for k in range(4):
                    tt = hb * 4 + k
                    for j in range(16):
                        op("pe", lambda: nc.tensor.matmul(bk[:, k * 128:(k + 1) * 128], lhsT=mergedT[:, j, tt * 128:(tt + 1) * 128],
                                                          rhs=wb[:, j * 128:(j + 1) * 128], start=(j == 0), stop=(j == 15)),
                           reads=[wbb, mg_b[j]], writes=[bb], signal=(j == 15 and k == 3))
                op("dve", lambda: nc.vector.tensor_tensor(out=xsl[u][:], in0=bk[:].rearrange("p (a k) -> p a k", k=128), in1=xsl[u][:], op=ALU.add),
                   reads=[bb, xsl_b[u]], writes=[xsl_b[u]])
                dma("sp", dview(x2s), xsl[u][:], reads=[xsl_b[u]], writes=[x2s_b])
        fw.barrier()
    st_mg.close(); st_xo.close(); st_br.close()
    if STAGE <= 4:
        with ExitStack() as sd:
            xt = sb("xt4", [128, 2048], F32, sd); xtb = Buf()
            for t in range(8):
                dma("sp", xt[:], x2s[t * 128:(t + 1) * 128, :], reads=[x2s_b], writes=[xtb])
                dma("sp", y[t * 128:(t + 1) * 128, :], xt[:], reads=[xtb])
            fw.barrier()
        es.close()
        return nc

    st_p = ExitStack()
    xn2T = sb("xn2T", [128, 16, 1024], BF16, st_p)
    xn2_b = [Buf() for _ in range(8)]
    IGG = sb("IGG", [128, 3, 8, 128], F32, st_p)
    igg_b = [Buf() for _ in range(8)]
    iota_f = sb("iota_f", [128, 128], F32, st_p); iota_b = Buf()
    dma("sp", iota_f[:], iota_d[:, :], writes=[iota_b])
    with ExitStack() as stn:
        norm_transpose(lambda tt: x2s[tt * 128:(tt + 1) * 128, :], lambda c0, t0, n: xn2T[:, c0:c0 + 8, t0:t0 + n], xn2_b, 8, stn, "n2")
        fw.barrier()
    xn2b = lambda ts: [xn2_b[t] for t in range(ts // 128, ts // 128 + 4)]
    with ExitStack() as sts:
        s_all = sb("s_all", [128, 8, 16, 128], F32, sts)
        sall_b = [Buf() for _ in range(8)]
        kst = sb("kst", [128, 16, 128], F32, sts); kst_b = Buf()
        kbf = sb("kbf", [128, 16, 128], BF16, sts)
        dma("sp", kst[:], keysT[:, :, :], writes=[kst_b])
        op("dve", lambda: nc.vector.tensor_copy(out=kbf[:], in_=kst[:]), reads=[kst_b], writes=[kst_b])
        qTp = [sb(f"qTp{i}", [128, 1024], BF16, sts) for i in range(2)]; qTp_b = [Buf(), Buf()]
        def q_scores(pair):
            s = pair % 2
            for hb in range(2):
                bk, bb = next_bank()
                for k in range(4):
                    tt = hb * 4 + k
                    op("pe", lambda: nc.tensor.matmul(bk[:, k * 128:(k + 1) * 128], lhsT=qTp[s][:, tt * 128:(tt + 1) * 128],
                                                      rhs=kbf[:, pair, :], start=True, stop=True),
                       reads=[qTp_b[s], kst_b], writes=[bb], signal=(k == 3))
                dst = s_all[:, hb * 4:hb * 4 + 4, pair, :]
                op("dve", lambda: nc.vector.tensor_copy(out=dst, in_=bk[:].rearrange("p (a k) -> p a k", k=128)),
                   reads=[bb], writes=sall_b[hb * 4:hb * 4 + 4])

        wqh = {0: wload(wq_g[0], scale=gffn), 1: wload(wq_g[1], scale=gffn)}
        for pair in range(16):
            s = pair % 2
            if pair + 2 < 16:
                wqh[pair + 2] = wload(wq_g[pair + 2], scale=gffn)
            wb, wbb = wqh.pop(pair)

            def ev_qp(bk, bb, ts, s=s):
                op("act", lambda: nc.scalar.copy(out=qTp[s][:, ts:ts + 512], in_=bk[:]), reads=[bb], writes=[qTp_b[s]])
            proj_fm(wb, wbb, xn2T, xn2b, 16, 0, 1024, ev_qp)
            if pair >= 1:
                q_scores(pair - 1)
        q_scores(15)
        it16 = sb("it16", [128, 16], F32, sts)
        op("dve", lambda: nc.vector.tensor_scalar(out=it16[:], in0=iota_f[:, 0:16], scalar1=16.0, scalar2=None, op0=ALU.mult),
           reads=[iota_b], writes=[iota_b])
        V = nc.vector

        def mk_scratch(i):
            d = {}
            d["sv"] = sb(f"sv{i}", [128, 16, 16], F32, sts); d["sv_b"] = Buf()
            d["si"] = sb(f"si{i}", [128, 16, 16], U32, sts)
            d["sif"] = sb(f"sif{i}", [128, 16, 16], F32, sts)
            d["sw"] = sb(f"sw{i}", [128, 128], F32, sts); d["sw_b"] = Buf()
            d["cand"] = sb(f"cand{i}", [128, 8, 16, 16], F32, sts); d["cand_b"] = Buf()
            d["cw"] = sb(f"cw{i}", [128, 256], F32, sts); d["cw_b"] = Buf()
            d["cv"] = sb(f"cv{i}", [128, 8, 16], F32, sts); d["cv_b"] = Buf()
            d["ci"] = sb(f"ci{i}", [128, 8, 16], U32, sts)
            d["cif"] = sb(f"cif{i}", [128, 8, 16], F32, sts)
            d["kf"] = sb(f"kf{i}", [128, 2, 8, 16], F32, sts); d["kf_b"] = Buf()
            d["ce"] = sb(f"ce{i}", [128, 8, 16], F32, sts); d["ce_b"] = Buf()
            d["zz"] = sb(f"zz5{i}", [128, 2, 8], F32, sts); d["zz_b"] = Buf()
            return d
        scr = [mk_scratch(0), mk_scratch(1)]

        def topk_tile(tt, d):
            sv, sv_b, si, sif, sw, sw_b = d["sv"], d["sv_b"], d["si"], d["sif"], d["sw"], d["sw_b"]
            cand, cand_b, cw, cw_b, cv, cv_b, ci, cif = d["cand"], d["cand_b"], d["cw"], d["cw_b"], d["cv"], d["cv_b"], d["ci"], d["cif"]
            kf, kf_b, ce, ce_b, zz, zz_b = d["kf"], d["kf_b"], d["ce"], d["ce_b"], d["zz"], d["zz_b"]
            eq, eq_b = cand, cand_b
            for p in range(16):
                src = s_all[:, tt, p, :]
                op("dve", lambda: V.max(out=sv[:, p, 0:8], in_=src), reads=[sall_b[tt]], writes=[sv_b]); yield
                op("dve", lambda: V.max_index(out=si[:, p, 0:8], in_max=sv[:, p, 0:8], in_values=src), reads=[sall_b[tt], sv_b], writes=[sv_b]); yield
                op("dve", lambda: V.match_replace(out=sw[:], in_to_replace=sv[:, p, 0:8], in_values=src, imm_value=NEG),
                   reads=[sall_b[tt], sv_b], writes=[sw_b]); yield
                op("dve", lambda: V.max(out=sv[:, p, 8:16], in_=sw[:]), reads=[sw_b], writes=[sv_b]); yield
                op("dve", lambda: V.max_index(out=si[:, p, 8:16], in_max=sv[:, p, 8:16], in_values=sw[:]), reads=[sw_b, sv_b], writes=[sv_b]); yield
            sv4 = sv[:].rearrange("p (h c) k -> p h c k", c=2)
            op("dve", lambda: V.tensor_tensor(out=cand[:], in0=sv4[:, :, 0, :].unsqueeze(3).to_broadcast([128, 8, 16, 16]),
                                              in1=sv4[:, :, 1, :].unsqueeze(2).to_broadcast([128, 8, 16, 16]), op=ALU.add),
               reads=[sv_b], writes=[cand_b]); yield
            for h in range(8):
                src = cand[:, h, :, :].rearrange("p a b -> p (a b)")
                op("dve", lambda: V.max(out=cv[:, h, 0:8], in_=src), reads=[cand_b], writes=[cv_b]); yield
                op("dve", lambda: V.max_index(out=ci[:, h, 0:8], in_max=cv[:, h, 0:8], in_values=src), reads=[cand_b, cv_b], writes=[cv_b]); yield
                op("dve", lambda: V.match_replace(out=cw[:], in_to_replace=cv[:, h, 0:8], in_values=src, imm_value=NEG),
                   reads=[cand_b, cv_b], writes=[cw_b]); yield
                op("dve", lambda: V.max(out=cv[:, h, 8:16], in_=cw[:]), reads=[cw_b], writes=[cv_b]); yield
                op("dve", lambda: V.max_index(out=ci[:, h, 8:16], in_max=cv[:, h, 8:16], in_values=cw[:]), reads=[cw_b, cv_b], writes=[cv_b]); yield
            op("dve", lambda: V.tensor_copy(out=cif[:], in_=ci[:]), reads=[cv_b], writes=[kf_b]); yield
            op("dve", lambda: V.tensor_tensor(out=eq[:], in0=cif[:].unsqueeze(3).to_broadcast([128, 8, 16, 16]),
                                              in1=it16[:].unsqueeze(1).unsqueeze(1).to_broadcast([128, 8, 16, 16]), op=ALU.is_ge),
               reads=[kf_b, cv_b], writes=[eq_b]); yield
            op("dve", lambda: V.tensor_reduce(out=kf[:, 0, :, :], in_=eq[:], axis=AX.X, op=ALU.add), reads=[eq_b], writes=[kf_b]); yield
            op("dve", lambda: V.tensor_scalar(out=kf[:, 0, :, :], in0=kf[:, 0, :, :], scalar1=-1.0, scalar2=None, op0=ALU.add),
               reads=[kf_b], writes=[kf_b]); yield
            op("dve", lambda: V.scalar_tensor_tensor(out=kf[:, 1, :, :], in0=kf[:, 0, :, :], scalar=-16.0, in1=cif[:], op0=ALU.mult, op1=ALU.add),
               reads=[kf_b], writes=[kf_b]); yield
            op("dve", lambda: V.tensor_copy(out=sif[:], in_=si[:]), reads=[sv_b], writes=[sv_b]); yield
            sif4 = sif[:].rearrange("p (h c) k -> p h c k", c=2)
            for c2 in range(2):
                op("dve", lambda: V.tensor_tensor(out=eq[:], in0=iota_f[:, 0:16].unsqueeze(1).unsqueeze(1).to_broadcast([128, 8, 16, 16]),
                                                  in1=kf[:, c2, :, :].unsqueeze(3).to_broadcast([128, 8, 16, 16]), op=ALU.is_equal),
                   reads=[kf_b, iota_b], writes=[eq_b]); yield
                op("dve", lambda: V.tensor_tensor(out=eq[:], in0=eq[:], in1=sif4[:, :, c2, :].unsqueeze(2).to_broadcast([128, 8, 16, 16]), op=ALU.mult),
                   reads=[eq_b, sv_b], writes=[eq_b]); yield
                op("dve", lambda: V.tensor_reduce(out=IGG[:, c2, tt, :].rearrange("p (h k) -> p h k", k=16), in_=eq[:], axis=AX.X, op=ALU.add),
                   reads=[eq_b], writes=[igg_b[tt]]); yield
            op("dve", lambda: V.tensor_tensor(out=ce[:], in0=cv[:], in1=cv[:, :, 0:1].to_broadcast([128, 8, 16]), op=ALU.subtract),
               reads=[cv_b], writes=[ce_b]); yield
            op("act", lambda: nc.scalar.activation(out=ce[:], in_=ce[:], func=AF.Exp), reads=[ce_b], writes=[ce_b]); yield
            op("dve", lambda: V.tensor_reduce(out=zz[:, 0, :], in_=ce[:], axis=AX.X, op=ALU.add), reads=[ce_b], writes=[zz_b]); yield
            op("dve", lambda: V.reciprocal(out=zz[:, 1, :], in_=zz[:, 0, :]), reads=[zz_b], writes=[zz_b]); yield
            op("dve", lambda: V.tensor_tensor(out=IGG[:, 2, tt, :].rearrange("p (h k) -> p h k", k=16), in0=ce[:],
                                              in1=zz[:, 1, :].unsqueeze(2).to_broadcast([128, 8, 16]), op=ALU.mult),
               reads=[ce_b, zz_b], writes=[igg_b[tt]]); yield

        for t2 in range(0, 8, 2):
            gens = [topk_tile(t2, scr[0]), topk_tile(t2 + 1, scr[1])]
            alive = [True, True]
            while any(alive):
                for gi_ in range(2):
                    if alive[gi_]:
                        try:
                            next(gens[gi_])
                        except StopIteration:
                            alive[gi_] = False
        fw.barrier()
    if "igg" in dbg_out:
        dma("sp", dbg_out["igg"][:, :, :, :], IGG[:], reads=igg_b)
        fw.barrier()
    with ExitStack() as stg:
        NQ = 32
        Aoh2 = [sb(f"Aoh{i}", [128, 128, NQ], BF16, stg) for i in range(2)]; A_b2 = [Buf(), Buf()]
        Boh2 = [sb(f"Boh{i}", [128, 128, NQ], BF16, stg) for i in range(2)]; B_b2 = [Buf(), Buf()]
        Gs2 = [sb(f"Gs{i}", [128, 128, 128], BF16, stg) for i in range(2)]; Gs_b2 = [Buf(), Buf()]
        iT2 = [sb(f"iT{i}", [128, 3, 128], BF16, stg) for i in range(2)]; iT_b2 = [Buf(), Buf()]
        iocol = sb("iocol", [128, 128, NQ], BF16, stg)
        op("dve", lambda: nc.vector.tensor_copy(out=iocol[:], in_=iota_f[:].unsqueeze(2).to_broadcast([128, 128, NQ])),
           reads=[iota_b], writes=[iota_b])
        qk = 0
        for tt in range(8):
            iT, iT_b = iT2[tt % 2], iT_b2[tt % 2]
            Gs, Gs_b = Gs2[tt % 2], Gs_b2[tt % 2]
            for q3 in range(3):
                bk, bb = next_bank()
                op("pe", lambda: nc.tensor.transpose(out=bk[:, 0:128], in_=IGG[:, q3, tt, :], identity=ident_f[:]),
                   reads=[igg_b[tt], ident_b], writes=[bb])
                op("act", lambda: nc.scalar.copy(out=iT[:, q3, :], in_=bk[:, 0:128]), reads=[bb], writes=[iT_b])
            for o in range(0, 128, NQ):
                u = qk % 2
                qk += 1
                Aoh, A_b, Boh, B_b = Aoh2[u], A_b2[u], Boh2[u], B_b2[u]
                bc = lambda q3: iT[:, q3, o:o + NQ].unsqueeze(1).to_broadcast([128, 128, NQ])
                op("dve", lambda: nc.vector.tensor_tensor(out=Aoh[:], in0=iocol[:], in1=bc(0), op=ALU.is_equal),
                   reads=[iT_b, iota_b], writes=[A_b])
                op("dve", lambda: nc.vector.tensor_tensor(out=Boh[:], in0=iocol[:], in1=bc(1), op=ALU.is_equal),
                   reads=[iT_b, iota_b], writes=[B_b])
                op("dve", lambda: nc.vector.tensor_tensor(out=Boh[:], in0=Boh[:], in1=bc(2), op=ALU.mult),
                   reads=[iT_b, B_b], writes=[B_b])
                for t0 in range(0, NQ, 4):
                    bk, bb = next_bank()
                    for j in range(4):
                        op("pe", lambda: nc.tensor.matmul(bk[:, j * 128:(j + 1) * 128], lhsT=Boh[:, :, t0 + j], rhs=Aoh[:, :, t0 + j],
                                                          start=True, stop=True), reads=[A_b, B_b], writes=[bb], signal=(j == 3))
                    src = bk[:].rearrange("p (j i) -> p i j", j=4)
                    dst = Gs[:, :, o + t0:o + t0 + 4]
                    op("act", lambda: nc.scalar.copy(out=dst, in_=src), reads=[bb], writes=[Gs_b])
            for c8 in range(8):
                dma("sp", gscr[c8 * 16:(c8 + 1) * 16, :, tt * 128:(tt + 1) * 128].rearrange("c p t -> p c t"),
                    Gs[:, c8 * 16:(c8 + 1) * 16, :], reads=[Gs_b], writes=[gscr_b])
        fw.barrier()
    x3 = sb("x3", [128, 8, 2048], F32, st_p)
    x3_b = [Buf() for _ in range(8)]
    for tt in range(8):
        dma("sp", x3[:, tt, :], x2s[tt * 128:(tt + 1) * 128, :], writes=[x3_b[tt]])
    NG = 8
    with ExitStack() as stm:
        AT = sb("AT", [128, NG, 1024], BF16, stm); AT_b = [Buf() for _ in range(NG)]
        vb = sb("vb", [128, NG, 2048], BF16, stm); vb_b = [Buf() for _ in range(NG)]
        gtmp = [sb(f"gtmp{i}", [128, 1024], BF16, stm) for i in range(2)]; gtmp_b = [Buf(), Buf()]
        gsl = [sb(f"gsl{i}", [128, 1024], BF16, stm) for i in range(2)]; gsl_b = [Buf() for _ in range(2)]
        NCH = 128
        SG = 4

        def U_part(c):
            cl = c % NG
            wb, wbb = wload(ut_g[c], scale=gffn)
            gi = c % 2
            dma("sp", gsl[gi][:], gscr[c, :, :], reads=[gscr_b], writes=[gsl_b[gi]])
            wload(v_g[c], dst=(vb[:, cl, :], vb_b[cl]))
            u = c % 2
            for ts in (0, 512):
                bk, bb = next_bank()
                for dc in range(16):
                    op("pe", lambda: nc.tensor.matmul(bk[:], lhsT=wb[:, dc * 128:(dc + 1) * 128], rhs=xn2T[:, dc, ts:ts + 512],
                                                      start=(dc == 0), stop=(dc == 15)), reads=[wbb] + xn2b(ts), writes=[bb], signal=(dc == 15))
                op("act", lambda: nc.scalar.activation(out=gtmp[u][:, ts:ts + 512], in_=bk[:], func=GELU), reads=[bb], writes=[gtmp_b[u]])
            op("dve", lambda: nc.vector.tensor_tensor(out=AT[:, cl, :], in0=gtmp[u][:], in1=gsl[gi][:], op=ALU.mult),
               reads=[gtmp_b[u], gsl_b[gi]], writes=[AT_b[cl]])

        def V_part(k):
            cls = [(k * SG + i) % NG for i in range(SG)]
            for tt in range(8):
                for dr in range(4):
                    bk, bb = next_bank()
                    for n_, ci_ in enumerate(cls):
                        op("pe", lambda: nc.tensor.matmul(bk[:], lhsT=AT[:, ci_, tt * 128:(tt + 1) * 128], rhs=vb[:, ci_, dr * 512:(dr + 1) * 512],
                                                          start=(n_ == 0), stop=(n_ == SG - 1)),
                           reads=[AT_b[ci_], vb_b[ci_]], writes=[bb], signal=(n_ == SG - 1))
                    dst = x3[:, tt, dr * 512:(dr + 1) * 512]
                    op("dve", lambda: nc.vector.tensor_tensor(out=dst, in0=bk[:], in1=dst, op=ALU.add), reads=[bb, x3_b[tt]], writes=[x3_b[tt]])

        nsub = NCH // SG
        for c in range(SG):
            U_part(c)
        for k in range(nsub):
            nxt = (k + 1) * SG
            if nxt < NCH:
                U_part(nxt)
            V_part(k)
            for c in range(nxt + 1, min(nxt + SG, NCH)):
                U_part(c)
        fw.barrier()
    with ExitStack() as stf:
        gf = sb("gf", [128, 2048], F32, stf); gf_b = Buf()
        dma("sp", gf[:], gfin_d[:, :], writes=[gf_b])
        jk = sb("jk", [128, 2048], BF16, stf); jk_b = Buf()
        ot = [sb(f"ot{i}", [128, 2048], F32, stf) for i in range(2)]; ot_b = [Buf(), Buf()]
        fs = sb("fs", [128, 8, 4], F32, stf); fs_b = [Buf() for _ in range(8)]
        for tt in range(8):
            u = tt % 2
            xa = x3[:, tt, :]
            op("act", lambda: nc.scalar.activation(out=jk[:], in_=xa, func=AF.Square, accum_out=fs[:, tt, 0:1]),
               reads=[x3_b[tt]], writes=[jk_b, fs_b[tt]])
            op("dve", lambda: nc.vector.tensor_scalar(out=fs[:, tt, 1:2], in0=fs[:, tt, 0:1], scalar1=1.0 / 2048, scalar2=1e-6,
                                                      op0=ALU.mult, op1=ALU.add), reads=[fs_b[tt]], writes=[fs_b[tt]])
            op("act", lambda: nc.scalar.activation(out=fs[:, tt, 2:3], in_=fs[:, tt, 1:2], func=AF.Sqrt), reads=[fs_b[tt]], writes=[fs_b[tt]])
            op("dve", lambda: nc.vector.reciprocal(out=fs[:, tt, 3:4], in_=fs[:, tt, 2:3]), reads=[fs_b[tt]], writes=[fs_b[tt]])
            op("dve", lambda: nc.vector.scalar_tensor_tensor(out=ot[u][:], in0=xa, scalar=fs[:, tt, 3:4], in1=gf[:], op0=ALU.mult, op1=ALU.mult),
               reads=[x3_b[tt], fs_b[tt], gf_b], writes=[ot_b[u]])
            dma("sp", y[tt * 128:(tt + 1) * 128, :], ot[u][:], reads=[ot_b[u]])
        fw.barrier()
    st_p.close()
    es.close()
    return nc


def rel_bucket_np(dist):
    n = np.maximum(dist, 0)
    max_exact = 16
    nf = np.maximum(n, 1).astype(np.float32)
    large = max_exact + (np.log(nf / max_exact) / math.log(128 / max_exact) * (32 - max_exact)).astype(np.int32)
    large = np.minimum(large, 31)
    return np.where(n < max_exact, n, large)


def make_inputs(inp):
    f = np.float32
    x = np.asarray(inp["x"], f)
    w_in = np.asarray(inp["w_in"], f)[0]
    cols = []
    for j in range(8):
        cols.append(4096 + j * 128)
    for j in range(8):
        cols.append(3072 + j * 128)
    for h in range(8):
        cols += [h * 128, 1024 + h * 128, 2048 + h * 128]
    for j in range(32):
        cols.append(5120 + j * 128)
    w4 = w_in.reshape(16, 128, 72, 128)
    nat = [c // 128 for c in cols]
    win_g = np.ascontiguousarray(w4[:, :, nat, :].transpose(2, 1, 0, 3)).reshape(72, 128, 2048)
    wbr = np.asarray(inp["w_branch"], f)[0]
    wbr_g = np.ascontiguousarray(wbr.reshape(2, 8, 128, 16, 128).transpose(0, 3, 2, 1, 4)).reshape(32, 128, 1024)
    wout = np.asarray(inp["w_out"], f)[0]
    wout_g = np.ascontiguousarray(wout.reshape(16, 128, 16, 128).transpose(2, 1, 0, 3)).reshape(16, 128, 2048)
    wq = np.asarray(inp["peer_wq"], f)[0]
    wq_g = np.ascontiguousarray(wq.reshape(16, 128, 16, 128).transpose(2, 1, 0, 3)).reshape(16, 128, 2048)
    U = np.asarray(inp["peer_u"], f)[0]
    ut_g = np.ascontiguousarray(U.reshape(128, 128, 16, 128).transpose(0, 3, 2, 1)).reshape(128, 128, 2048)
    v_g = np.ascontiguousarray(np.asarray(inp["peer_v"], f)[0].reshape(128, 128, 2048))
    keys = np.asarray(inp["peer_keys"], f)[0]
    keysT = np.ascontiguousarray(keys.reshape(16, 128, 128).transpose(2, 0, 1))
    cst = np.zeros((128, 512), f)
    cst[:, 0:16] = np.asarray(inp["norm_mix_g"], f)[0].reshape(16, 128).T
    cst[:, 16:32] = np.asarray(inp["norm_ffn_g"], f)[0].reshape(16, 128).T
    cw = np.asarray(inp["conv_w"], f)[0]
    cst[:, 32:64] = cw.reshape(4, 8, 128).transpose(2, 1, 0).reshape(128, 32)
    cst[:, 64:72] = np.asarray(inp["conv_b"], f)[0].reshape(8, 128).T
    cst[:, 72:80] = np.asarray(inp["lru_ba"], f)[0].reshape(8, 128).T
    cst[:, 80:88] = np.asarray(inp["lru_bx"], f)[0].reshape(8, 128).T
    cst[:, 88:96] = np.asarray(inp["lru_lambda"], f)[0].reshape(8, 128).T
    rb = np.asarray(inp["rel_bias"], f)
    cst[:, 100:108] = rb[31][None, :]
    lruw = np.stack([np.asarray(inp["lru_wa"], f)[0], np.asarray(inp["lru_wx"], f)[0]], 0)
    lruw = np.ascontiguousarray(lruw.transpose(2, 0, 1, 3))
    ident = np.eye(128, dtype=f)
    qi = np.arange(128)[:, None]
    ki = np.arange(256)[None, :]
    ownbias = np.zeros((128, 8, 2, 256), f)
    for par in range(2):
        dist = par * 128 + qi - ki
        bk = rel_bucket_np(dist)
        vals = rb[bk]
        vals = np.where((dist >= 0)[:, :, None], vals, f(NEG))
        ownbias[:, :, par, :] = vals.transpose(0, 2, 1)
    distp = 256 + qi - ki
    prevbias = np.ascontiguousarray(rb[rel_bucket_np(distp)].transpose(0, 2, 1))
    gfin = np.broadcast_to(np.asarray(inp["norm_final_g"], f)[None, :], (128, 2048)).copy()
    iota = np.broadcast_to(np.arange(128, dtype=f)[None, :], (128, 128)).copy()
    if DEBUG.get("stage", 99) < 5:
        ut_g, v_g = ut_g[:1], v_g[:1]
    shared = dict(win_g=win_g, wbr_g=wbr_g, wout_g=wout_g, wq_g=wq_g, ut_g=ut_g, v_g=v_g, keysT=keysT,
                  lruw=lruw, ident=ident, ownbias=ownbias, prevbias=prevbias, gfin=gfin, iota=iota)
    in_maps = []
    for core in range(8):
        b, half = core // 2, core % 2
        xl = np.zeros((2048, 2048), f)
        if half == 1:
            xl[:] = x[b]
        else:
            xl[1024:] = x[b, :1024]
        c2 = cst.copy()
        c2[:, 96] = float(half)
        bv = np.full((128, 8, 8), NEG, f)
        for qt in range(8):
            ob = 4 + qt // 2
            lo = 0 if half == 1 else 4
            bv[:, qt, lo:ob] = 0.0
        m = dict(shared)
        m.update(xloc=xl, cst=c2, blkvalid=bv)
        in_maps.append(m)
    return in_maps


def kernel(**inputs):
    dbg = DEBUG.get("dbg")
    nc = build_program(dbg)
    in_maps = make_inputs(inputs)
    res = run_bass_kernel_spmd(nc, in_maps, core_ids=list(range(8)))
    if dbg:
        DEBUG["res"] = res.results
    out = np.zeros((4, 2048, 2048), np.float32)
    for core in range(8):
        b, half = core // 2, core % 2
        out[b, half * 1024:(half + 1) * 1024] = res.results[core]["y"]
    return out
```
